# Optimizing a Trainium2 kernel written in Bass

```python
import math
import jax, jax.numpy as jnp
from jax import lax
import numpy as np

D_MODEL = 1024
BATCH = 2
SEQ = 8192
DEPTH = 1

MIX_WIDTH = D_MODEL
MLA_WIDTH = MIX_WIDTH // 2
HYENA_WIDTH = MIX_WIDTH - MLA_WIDTH
MLA_HEADS = 8
MLA_V_DIM = MLA_WIDTH // MLA_HEADS
MLA_NOPE_DIM = 64
MLA_ROPE_DIM = 32
Q_LORA_RANK = D_MODEL // 4
KV_LORA_RANK = D_MODEL // 8
ROPE_THETA = 10000.0
Q_BLOCK = 128
HYENA_ORDER = 2
SHORT_CONV = 3
FILTER_EMB = 33
FILTER_ORDER = 64
DECAY_TARGET = 1e-2
FAST_DECAY_PCT = 0.3
SLOW_DECAY_PCT = 1.5
Q_A_COLS = Q_LORA_RANK
KV_A_COLS = KV_LORA_RANK + MLA_ROPE_DIM
HY_COLS = (HYENA_ORDER + 1) * HYENA_WIDTH
IN_COLS = Q_A_COLS + KV_A_COLS + HY_COLS
N_EXPERTS = 16
EC_FACTOR = 2
EXPERT_FF = D_MODEL
DN_ALPHA = (2.0 * DEPTH) ** 0.25
DN_BETA = (8.0 * DEPTH) ** -0.25
EPS = 1e-5

kernel_name = "hybrid_mla_hyena_ecmoe_deepnorm_adaln"


def layer_norm(x, g, b):
    xf = x.astype(jnp.float32)
    mu = jnp.mean(xf, axis=-1, keepdims=True)
    var = jnp.mean(jnp.square(xf - mu), axis=-1, keepdims=True)
    return ((xf - mu) * lax.rsqrt(var + EPS) * g + b).astype(x.dtype)


def rms_norm(x, g):
    xf = x.astype(jnp.float32)
    return (xf * lax.rsqrt(jnp.mean(xf * xf, axis=-1, keepdims=True) + EPS) * g).astype(x.dtype)


def apply_rope(x, cos, sin):
    half = MLA_ROPE_DIM // 2
    x1, x2 = x[..., :half], x[..., half:]
    return jnp.concatenate([x1 * cos - x2 * sin, x2 * cos + x1 * sin], axis=-1).astype(x.dtype)


def mla_group(cq_raw, ckv_raw, positions, q_norm_g, w_qb, kv_norm_g, w_kvb):
    B, S, _ = cq_raw.shape
    H = MLA_HEADS
    q = (rms_norm(cq_raw, q_norm_g) @ w_qb).reshape(B, S, H, MLA_NOPE_DIM + MLA_ROPE_DIM)
    q_nope, q_rope = q[..., :MLA_NOPE_DIM], q[..., MLA_NOPE_DIM:]
    c_kv, k_rope = ckv_raw[..., :KV_LORA_RANK], ckv_raw[..., KV_LORA_RANK:]
    kv = (rms_norm(c_kv, kv_norm_g) @ w_kvb).reshape(B, S, H, MLA_NOPE_DIM + MLA_V_DIM)
    k_nope, v = kv[..., :MLA_NOPE_DIM], kv[..., MLA_NOPE_DIM:]
    half = MLA_ROPE_DIM // 2
    inv_freq = ROPE_THETA ** (-jnp.arange(half, dtype=jnp.float32) / half)
    ang = positions.astype(jnp.float32)[..., None] * inv_freq
    cos, sin = jnp.cos(ang), jnp.sin(ang)
    q_rope = apply_rope(q_rope, cos[:, :, None, :], sin[:, :, None, :])
    k_rope = apply_rope(k_rope, cos, sin)
    scale = (MLA_NOPE_DIM + MLA_ROPE_DIM) ** -0.5
    nb = S // Q_BLOCK
    qn_b = q_nope.reshape(B, nb, Q_BLOCK, H, MLA_NOPE_DIM).transpose(1, 0, 2, 3, 4)
    qr_b = q_rope.reshape(B, nb, Q_BLOCK, H, MLA_ROPE_DIM).transpose(1, 0, 2, 3, 4)

    def attend(blk):
        qn, qr = blk
        s = jnp.einsum('bqhd,bkhd->bhqk', qn, k_nope) + jnp.einsum('bqhr,bkr->bhqk', qr, k_rope)
        p = jax.nn.softmax(s.astype(jnp.float32) * scale, axis=-1).astype(v.dtype)
        return jnp.einsum('bhqk,bkhd->bqhd', p, v)

    o = lax.map(attend, (qn_b, qr_b))
    return o.transpose(1, 0, 2, 3, 4).reshape(B, S, H * MLA_V_DIM)


def hyena_filter_spectrum(L, w1, b1, freq, w2, b2, w3, b3, decay):
    f32 = jnp.float32
    pos = jnp.arange(L, dtype=f32)
    t = pos[:, None] / (L - 1)
    bands = (FILTER_EMB - 1) // 2
    freqs = jnp.linspace(1e-4, bands - 1, bands, dtype=f32)
    phase = (2.0 * math.pi / L) * pos[:, None] * freqs[None, :]
    feats = jnp.concatenate([t, jnp.cos(phase), -jnp.sin(phase)], axis=-1)
    fr = freq.astype(f32)
    h = jnp.sin(fr * (feats @ w1.astype(f32) + b1.astype(f32)))
    h = jnp.sin(fr * (h @ w2.astype(f32) + b2.astype(f32)))
    h = (h @ w3.astype(f32) + b3.astype(f32)).reshape(L, 2, HYENA_ORDER, HYENA_WIDTH)
    h = h * jnp.exp(-t[:, :, None, None] * jnp.abs(decay.astype(f32)))
    h_fwd, h_bwd = h[:, 0], h[:, 1]
    two_sided = jnp.concatenate([h_fwd, jnp.zeros_like(h_fwd[:1]), h_bwd[:0:-1]], axis=0)
    return jnp.fft.rfft(two_sided, axis=0)


def hyena_group(hy_raw, conv_w, conv_b, filt_f, hyena_bias):
    B, S, _ = hy_raw.shape
    pad = SHORT_CONV // 2
    xp = jnp.pad(hy_raw, ((0, 0), (pad, pad), (0, 0)))
    u = conv_b + sum(xp[:, j:j + S] * conv_w[j] for j in range(SHORT_CONV))
    parts = jnp.split(u, HYENA_ORDER + 1, axis=-1)
    z = parts[0]
    for o in range(HYENA_ORDER):
        zf = z.astype(jnp.float32)
        y = jnp.fft.irfft(jnp.fft.rfft(zf, n=2 * S, axis=1) * filt_f[:, o], n=2 * S, axis=1)[:, :S]
        z = (parts[o + 1].astype(jnp.float32) * (y + zf * hyena_bias[o])).astype(hy_raw.dtype)
    return z


def expert_choice_moe(u, w_router, w_gate, w_up, w_down):
    B, S, D = u.shape
    cap = EC_FACTOR * S // N_EXPERTS
    aff = jax.nn.softmax((u @ w_router).astype(jnp.float32), axis=-1)
    g, idx = lax.top_k(aff.transpose(0, 2, 1), cap)
    xe = jax.vmap(lambda ub, ib: ub[ib])(u, idx)
    h = jax.nn.silu(jnp.einsum('becd,edf->becf', xe, w_gate)) * jnp.einsum('becd,edf->becf', xe, w_up)
    ye = jnp.einsum('becf,efd->becd', h, w_down) * g[..., None].astype(u.dtype)
    flat_idx = (idx + (jnp.arange(B, dtype=idx.dtype) * S)[:, None, None]).reshape(-1)
    out = jnp.zeros((B * S, D), u.dtype).at[flat_idx].add(ye.reshape(-1, D))
    return out.reshape(B, S, D)


def setup_inputs(seed: int = 0) -> dict:
    key = jax.random.key(seed)
    ks = iter(jax.random.split(key, 40))
    f32 = jnp.float32
    L_ = DEPTH

    def nrm(shape, std):
        return jax.random.normal(next(ks), shape, f32) * std

    def gain(shape):
        return 1.0 + nrm(shape, 0.02)

    x = jax.random.normal(next(ks), (BATCH, SEQ, D_MODEL), f32)
    c = jax.random.normal(next(ks), (BATCH, D_MODEL), f32)
    offs = jax.random.randint(next(ks), (BATCH, 1), 0, 4096, dtype=jnp.int32)
    positions = offs + jnp.arange(SEQ, dtype=jnp.int32)[None, :]
    min_decay = abs(math.log(DECAY_TARGET) / SLOW_DECAY_PCT)
    max_decay = abs(math.log(DECAY_TARGET) / FAST_DECAY_PCT)
    base_decay = jnp.linspace(min_decay, max_decay, HYENA_WIDTH, dtype=f32)
    hyena_decay = base_decay * jnp.exp(nrm((L_, 2, HYENA_ORDER, HYENA_WIDTH), 0.1))
    return {
        "x": x,
        "c": c,
        "positions": positions,
        "w_ada": nrm((L_, D_MODEL, 6 * D_MODEL), 0.5 * D_MODEL ** -0.5),
        "b_ada": nrm((L_, 6 * D_MODEL), 0.01),
        "w_in": nrm((L_, D_MODEL, IN_COLS), D_MODEL ** -0.5),
        "q_norm_g": gain((L_, Q_LORA_RANK)),
        "w_qb": nrm((L_, Q_LORA_RANK, MLA_HEADS * (MLA_NOPE_DIM + MLA_ROPE_DIM)), Q_LORA_RANK ** -0.5),
        "kv_norm_g": gain((L_, KV_LORA_RANK)),
        "w_kvb": nrm((L_, KV_LORA_RANK, MLA_HEADS * (MLA_NOPE_DIM + MLA_V_DIM)), KV_LORA_RANK ** -0.5),
        "conv_w": nrm((L_, SHORT_CONV, HY_COLS), SHORT_CONV ** -0.5),
        "conv_b": nrm((L_, HY_COLS), 0.01),
        "filt_w1": nrm((L_, FILTER_EMB, FILTER_ORDER), FILTER_EMB ** -0.5),
        "filt_b1": nrm((L_, FILTER_ORDER), 0.02),
        "filt_freq": gain((L_, FILTER_ORDER)),
        "filt_w2": nrm((L_, FILTER_ORDER, FILTER_ORDER), FILTER_ORDER ** -0.5),
        "filt_b2": nrm((L_, FILTER_ORDER), 0.02),
        "filt_w3": nrm((L_, FILTER_ORDER, 2 * HYENA_ORDER * HYENA_WIDTH), 0.1 * FILTER_ORDER ** -0.5),
        "filt_b3": nrm((L_, 2 * HYENA_ORDER * HYENA_WIDTH), 0.01),
        "hyena_decay": hyena_decay,
        "hyena_bias": nrm((L_, HYENA_ORDER, HYENA_WIDTH), 1.0),
        "attn_out_g": gain((L_, MLA_WIDTH)),
        "hyena_out_g": gain((L_, HYENA_WIDTH)),
        "w_o": nrm((L_, MIX_WIDTH, D_MODEL), DN_BETA * MIX_WIDTH ** -0.5),
        "ln1_g": gain((L_, D_MODEL)),
        "ln1_b": nrm((L_, D_MODEL), 0.01),
        "w_router": nrm((L_, D_MODEL, N_EXPERTS), D_MODEL ** -0.5),
        "w_gate": nrm((L_, N_EXPERTS, D_MODEL, EXPERT_FF), D_MODEL ** -0.5),
        "w_up": nrm((L_, N_EXPERTS, D_MODEL, EXPERT_FF), D_MODEL ** -0.5),
        "w_down": nrm((L_, N_EXPERTS, EXPERT_FF, D_MODEL), DN_BETA * EXPERT_FF ** -0.5),
        "ln2_g": gain((L_, D_MODEL)),
        "ln2_b": nrm((L_, D_MODEL), 0.01),
    }


def reference(x, c, positions, w_ada, b_ada, w_in, q_norm_g, w_qb, kv_norm_g, w_kvb,
              conv_w, conv_b, filt_w1, filt_b1, filt_freq, filt_w2, filt_b2, filt_w3, filt_b3,
              hyena_decay, hyena_bias, attn_out_g, hyena_out_g, w_o, ln1_g, ln1_b,
              w_router, w_gate, w_up, w_down, ln2_g, ln2_b):
    S = x.shape[1]
    for l in range(DEPTH):
        mod = jax.nn.silu(c) @ w_ada[l] + b_ada[l]
        sh1, sc1, g1, sh2, sc2, g2 = jnp.split(mod[:, None, :], 6, axis=-1)
        u = x * (1.0 + sc1) + sh1
        proj = u @ w_in[l]
        cq_raw = proj[..., :Q_A_COLS]
        ckv_raw = proj[..., Q_A_COLS:Q_A_COLS + KV_A_COLS]
        hy_raw = proj[..., Q_A_COLS + KV_A_COLS:]
        a = mla_group(cq_raw, ckv_raw, positions, q_norm_g[l], w_qb[l], kv_norm_g[l], w_kvb[l])
        filt_f = hyena_filter_spectrum(S, filt_w1[l], filt_b1[l], filt_freq[l], filt_w2[l], filt_b2[l],
                                       filt_w3[l], filt_b3[l], hyena_decay[l])
        hy = hyena_group(hy_raw, conv_w[l], conv_b[l], filt_f, hyena_bias[l])
        mixed = jnp.concatenate([rms_norm(a, attn_out_g[l]), rms_norm(hy, hyena_out_g[l])], axis=-1) @ w_o[l]
        x = layer_norm(DN_ALPHA * x + g1 * mixed, ln1_g[l], ln1_b[l])
        u = x * (1.0 + sc2) + sh2
        ffn = expert_choice_moe(u, w_router[l], w_gate[l], w_up[l], w_down[l])
        x = layer_norm(DN_ALPHA * x + g2 * ffn, ln2_g[l], ln2_b[l])
    return x
```

```python
from contextlib import ExitStack
import concourse.bass as bass
import concourse.mybir as mybir

_DT_SIZE = {"float32": 4, "bfloat16": 2, "int32": 4, "uint32": 4, "float16": 2, "uint8": 1, "int8": 1,
            "uint16": 2, "int16": 2}


def _dsize(dt):
    n = getattr(dt, "name", None) or str(dt)
    for k_, v in _DT_SIZE.items():
        if k_ in str(n):
            return v
    raise ValueError(f"dtype {dt}")


class Buf:
    __slots__ = ("name", "t", "last_w", "readers")

    def __init__(self, name, t=None):
        self.name = name
        self.t = t
        self.last_w = None
        self.readers = []


class Op:
    __slots__ = ("eng", "fn", "deps", "kind", "awaited", "sem", "semval", "idx", "final", "dclass")

    def __init__(self, eng, fn, kind):
        self.eng = eng
        self.fn = fn
        self.deps = []
        self.kind = kind
        self.awaited = False
        self.sem = None
        self.semval = None
        self.final = False
        self.dclass = None


ENGS = ["sync", "scalar", "vector", "gpsimd", "tensor"]
NDMA = 8
import os
CC_INC = int(os.environ.get("CC_INC", "1"))


class K:
    def __init__(self, nc, sbuf_base=16640, sbuf_limit=None):
        self.nc = nc
        self.ops = {e: [] for e in ENGS}
        self.stack = ExitStack()
        self.sb_off = sbuf_base
        self.sb_limit = sbuf_limit if sbuf_limit is not None else 16512 + nc.sbuf_bytes_remaining
        self.sb_hw = sbuf_base
        self.sb_epoch = 0
        self.dma_count = {e: 0 for e in ENGS}
        self.dma_last = {}
        self.barrier_ops = {e: [] for e in ENGS}
        self.finals = []
        self.n_names = 0
        self.ps_live = []

    def sb(self, name, shape, dtype):
        nbytes = _dsize(dtype)
        for s in shape[1:]:
            nbytes *= s
        off = (self.sb_off + 63) // 64 * 64
        assert off + nbytes <= self.sb_limit, f"SBUF overflow at {name}: {off}+{nbytes}"
        self.n_names += 1
        t = self.nc.alloc_sbuf_tensor_at(f"{name}_{self.n_names}", list(shape), dtype, offset=off)
        self.sb_off = off + nbytes
        self.sb_hw = max(self.sb_hw, self.sb_off)
        return Buf(name, t)

    def mark(self):
        return self.sb_off

    def release(self, mark):
        self.sb_off = mark
        self.sb_epoch += 1
        self.barrier()

    def ps(self, name, shape, dtype):
        self.n_names += 1
        t = self.stack.enter_context(self.nc.psum_tensor(f"{name}_{self.n_names}", list(shape), dtype))
        return Buf(name, t)

    def buf(self, name):
        return Buf(name)

    def _add(self, op, r, w):
        eng = op.eng
        deps = []
        for b in r:
            if b.last_w is not None:
                deps.append(b.last_w)
        for b in w:
            if b.last_w is not None:
                deps.append(b.last_w)
            for rd in b.readers:
                deps.append(rd)
        if self.barrier_ops[eng]:
            deps.extend(self.barrier_ops[eng])
            self.barrier_ops[eng] = []
        seen = set()
        for d in deps:
            if d is op or id(d) in seen:
                continue
            seen.add(id(d))
            if d.eng == eng and d.kind == "compute":
                if eng == "tensor":
                    continue
            op.deps.append(d)
        for b in r:
            b.readers.append(op)
        for b in w:
            b.last_w = op
            b.readers = []
        self.ops[eng].append(op)
        return op

    def op(self, eng, fn, r=(), w=()):
        return self._add(Op(eng, fn, "compute"), list(r), list(w))

    def dma(self, eng, out, in_, r=(), w=(), final=False, **kw):
        op = Op(eng, lambda e: e.dma_start(out=out, in_=in_, **kw), "dma")
        cls = self.dma_count[eng] % NDMA
        self.dma_count[eng] += 1
        op.dclass = cls
        prev = self.dma_last.get((eng, cls))
        self._add(op, list(r), list(w))
        if prev is not None and prev not in op.deps:
            op.deps.append(prev)
        self.dma_last[(eng, cls)] = op
        if final:
            op.final = True
            self.finals.append(op)
        return op

    def custom_dma(self, eng, fn, r=(), w=(), final=False):
        op = Op(eng, fn, "dma")
        cls = self.dma_count[eng] % NDMA
        self.dma_count[eng] += 1
        op.dclass = cls
        prev = self.dma_last.get((eng, cls))
        self._add(op, list(r), list(w))
        if prev is not None and prev not in op.deps:
            op.deps.append(prev)
        self.dma_last[(eng, cls)] = op
        if final:
            op.final = True
            self.finals.append(op)
        return op

    def collective(self, kind, alu, groups, ins, outs, r=(), w=()):
        op = Op("gpsimd", lambda e: e.collective_compute(kind, alu, replica_groups=groups, ins=ins, outs=outs), "cc")
        self._add(op, list(r), list(w))
        return op

    def barrier(self):
        lasts = []
        for e in ENGS:
            if self.ops[e]:
                lasts.append(self.ops[e][-1])
        for key, op in self.dma_last.items():
            lasts.append(op)
        for e in ENGS:
            self.barrier_ops[e] = list(lasts)

    def emit(self):
        nc = self.nc
        tail_deps = list(self.finals)
        for d in tail_deps:
            d.awaited = True
        for e in ENGS:
            for op in self.ops[e]:
                for d in op.deps:
                    d.awaited = True
        with ExitStack() as st:
            esem = {e: st.enter_context(nc.semaphore(f"s_{e}")) for e in ENGS}
            dsem = {(e, c): st.enter_context(nc.semaphore(f"d_{e}_{c}")) for e in ENGS if self.dma_count[e] > 0
                    for c in range(min(NDMA, self.dma_count[e]))}
            for e in ENGS:
                cnt = 0
                dcnt = {}
                for op in self.ops[e]:
                    if op.kind == "compute":
                        if op.awaited:
                            cnt += 1
                            op.sem = esem[e]
                            op.semval = cnt
                    elif op.kind == "cc":
                        op.sem = st.enter_context(nc.semaphore(f"cc_{id(op)}"))
                        op.semval = CC_INC
                        op.awaited = True
                    else:
                        c = op.dclass
                        dcnt[c] = dcnt.get(c, 0) + 16
                        op.sem = dsem[(e, c)]
                        op.semval = dcnt[c]
                        op.awaited = True
            block = st.enter_context(nc.Block())
            handles = {"sync": block.sync, "scalar": block.scalar, "vector": block.vector,
                       "gpsimd": block.gpsimd, "tensor": block.tensor}

            def make(e):
                def body(eng):
                    seen = {}
                    for op in self.ops[e]:
                        for d in op.deps:
                            key = id(d.sem)
                            if seen.get(key, 0) >= d.semval:
                                continue
                            eng.wait_ge(d.sem, d.semval)
                            seen[key] = d.semval
                        ins = op.fn(eng)
                        if op.kind == "compute":
                            if op.awaited:
                                ins.then_inc(op.sem, 1)
                        elif op.kind == "cc":
                            ins.then_inc(op.sem, CC_INC)
                        else:
                            ins.then_inc(op.sem, 16)
                    if e == "sync":
                        for d in tail_deps:
                            key = id(d.sem)
                            if seen.get(key, 0) >= d.semval:
                                continue
                            eng.wait_ge(d.sem, d.semval)
                            seen[key] = d.semval
                return body

            for e in ENGS:
                if self.ops[e] or e == "sync":
                    handles[e](make(e))
        self.stack.close()


import os
import numpy as np
import ml_dtypes
import concourse.bass as bass
import concourse.mybir as mybir
from concourse.bass_utils import run_bass_kernel_spmd

F32 = mybir.dt.float32
BF16 = mybir.dt.bfloat16
I32 = mybir.dt.int32
AF = mybir.ActivationFunctionType
ALU = mybir.AluOpType
AX = mybir.AxisListType

D = 1024
S = 8192
SO = 2048
H = 8
EPS = 1e-5
ALPHA = 2.0 ** 0.25
PI = float(np.pi)
MAGIC = 12582912.0
SCALE = 96.0 ** -0.5
NFFT = 16384


def host_consts():
    c = {}
    n = np.arange(128, dtype=np.float64)
    ang128 = 2 * np.pi * np.outer(n, n) / 128.0
    C2 = np.cos(ang128)
    S2 = np.sin(ang128)
    c["F1"] = np.concatenate([C2, -S2], 1).astype(np.float32)
    c["C2"] = C2.astype(np.float32)
    c["S2"] = S2.astype(np.float32)
    c["S2n"] = (-S2).astype(np.float32)
    c["G1"] = np.concatenate([C2, S2], 1).astype(np.float32)
    c["G2"] = np.concatenate([-S2, C2], 1).astype(np.float32)
    th = 2 * np.pi * np.outer(n, n) / NFFT
    c["TC"] = np.cos(th).astype(np.float32)
    c["TS"] = np.sin(th).astype(np.float32)
    c["TSn"] = (-np.sin(th)).astype(np.float32)
    c["CI"] = (C2[:, :64] / NFFT).astype(np.float32)
    c["SIn"] = (-S2[:, :64] / NFFT).astype(np.float32)
    L = S
    m = np.arange(NFFT)
    lag = np.where(m < L, m, NFFT - m)
    lag = np.where(m == L, 0, lag)
    pos = lag.astype(np.float32)
    t = pos / np.float32(L - 1)
    bands = 16
    freqs = np.linspace(1e-4, bands - 1, bands, dtype=np.float32)
    phase = (np.float32(2.0 * np.pi / L) * pos[:, None]) * freqs[None, :]
    feats = np.concatenate([t[:, None], np.cos(phase), -np.sin(phase)], -1).astype(np.float32)
    c["featsT"] = np.ascontiguousarray(feats.T)
    n1 = np.arange(128)
    e1 = np.where(n1 < 64, 128.0 * n1, NFFT - 128.0 * n1) / (L - 1)
    c["e1s"] = (-e1).astype(np.float32).reshape(128, 1)
    c["n2row"] = np.broadcast_to(np.arange(128, dtype=np.float32)[None, :], (128, 128)).copy()
    inv_freq = 10000.0 ** (-np.arange(16, dtype=np.float32) / 16.0)
    invf = np.zeros((128, 1), np.float32)
    sgn = np.zeros((128, 1), np.float32)
    invf[64:80, 0] = inv_freq
    invf[80:96, 0] = inv_freq
    sgn[64:80, 0] = -1.0
    sgn[80:96, 0] = 1.0
    c["invf"] = invf
    c["sgn"] = sgn
    return c


def fm(v, k):
    return np.ascontiguousarray(np.asarray(v).reshape(k, 128).T)


def host_prep(inp, core):
    b, j = core // 4, core % 4
    l = 0
    o = {}
    o["xb"] = np.ascontiguousarray(inp["x"][b])
    o["cvec"] = fm(inp["c"][b], 8)
    o["posf"] = np.ascontiguousarray(inp["positions"][b].reshape(1, S).astype(np.int32))
    o["w_ada"] = np.ascontiguousarray(inp["w_ada"][l])
    o["b_ada"] = np.ascontiguousarray(inp["b_ada"][l].reshape(1, 6 * D))
    w_in = inp["w_in"][l]
    W1 = np.zeros((D, 768), np.float32)
    W1[:, 0:128] = w_in[:, 256:384]
    W1[:, 128 + 64:128 + 96] = w_in[:, 384:416]
    W1[:, 256 + 64:256 + 80] = w_in[:, 400:416]
    W1[:, 256 + 80:256 + 96] = w_in[:, 384:400]
    for g in range(3):
        c0 = 416 + g * 512 + 128 * j
        W1[:, 384 + g * 128:384 + (g + 1) * 128] = w_in[:, c0:c0 + 128]
    o["W1"] = W1
    o["wcq"] = np.ascontiguousarray(w_in[:, 0:256])
    o["qg"] = fm(inp["q_norm_g"][l], 2)
    wqb = inp["w_qb"][l]
    wq = np.zeros((256, H, 2, 128), np.float32)
    for h in range(H):
        wq[:, h, 0, 0:64] = wqb[:, h * 96:h * 96 + 64]
        wq[:, h, 0, 64:96] = wqb[:, h * 96 + 64:h * 96 + 96]
        wq[:, h, 1, 64:80] = wqb[:, h * 96 + 80:h * 96 + 96]
        wq[:, h, 1, 80:96] = wqb[:, h * 96 + 64:h * 96 + 80]
    o["wq"] = wq.reshape(256, H * 2 * 128)
    o["kvg"] = fm(inp["kv_norm_g"][l], 1)
    wkvb = inp["w_kvb"][l]
    wk = np.zeros((128, H, 128), np.float32)
    wv = np.zeros((128, H, 64), np.float32)
    for h in range(H):
        wk[:, h, 0:64] = wkvb[:, h * 128:h * 128 + 64]
        wv[:, h, :] = wkvb[:, h * 128 + 64:h * 128 + 128]
    o["wk"] = wk.reshape(128, H * 128)
    o["wv"] = wv.reshape(128, H * 64)
    cw = inp["conv_w"][l]
    cb = inp["conv_b"][l]
    cwl = np.zeros((128, 3, 3), np.float32)
    cbl = np.zeros((128, 3), np.float32)
    for g in range(3):
        sl = slice(g * 512 + 128 * j, g * 512 + 128 * j + 128)
        cwl[:, g, :] = cw[:, sl].T
        cbl[:, g] = cb[sl]
    o["cw"] = cwl.reshape(128, 9)
    o["cb"] = cbl
    o["fw1"] = np.ascontiguousarray(inp["filt_w1"][l])
    o["fb1"] = np.ascontiguousarray(inp["filt_b1"][l].reshape(64, 1))
    o["ffreq"] = np.ascontiguousarray(inp["filt_freq"][l].reshape(64, 1))
    o["fw2"] = np.ascontiguousarray(inp["filt_w2"][l])
    o["fb2"] = np.ascontiguousarray(inp["filt_b2"][l].reshape(64, 1))
    w3 = inp["filt_w3"][l].reshape(64, 2, 2, 512)
    b3 = inp["filt_b3"][l].reshape(2, 2, 512)
    dec = inp["hyena_decay"][l]
    cs = slice(128 * j, 128 * j + 128)
    w3l = w3[:, :, :, cs].reshape(64, 2, 2, 4, 32).transpose(0, 2, 3, 1, 4)
    o["fw3"] = np.ascontiguousarray(w3l.reshape(64, 512))
    b3l = b3[:, :, cs].reshape(2, 2, 4, 32).transpose(1, 2, 0, 3)
    o["fb3"] = np.ascontiguousarray(b3l.reshape(1, 512))
    dl = dec[:, :, cs].reshape(2, 2, 4, 32).transpose(1, 2, 0, 3)
    o["fdec"] = np.ascontiguousarray(dl.reshape(1, 512))
    o["hbias"] = np.ascontiguousarray(inp["hyena_bias"][l][:, cs].reshape(1, 256))
    o["ag"] = np.ascontiguousarray(inp["attn_out_g"][l].reshape(1, 512))
    o["hg"] = fm(inp["hyena_out_g"][l], 4)
    o["w_o"] = np.ascontiguousarray(inp["w_o"][l])
    o["ln1g"] = np.ascontiguousarray(inp["ln1_g"][l].reshape(1, D))
    o["ln1b"] = np.ascontiguousarray(inp["ln1_b"][l].reshape(1, D))
    o["wr"] = np.ascontiguousarray(inp["w_router"][l])
    o["wg"] = np.ascontiguousarray(inp["w_gate"][l][4 * j:4 * j + 4])
    o["wu"] = np.ascontiguousarray(inp["w_up"][l][4 * j:4 * j + 4])
    o["wd"] = np.ascontiguousarray(inp["w_down"][l][4 * j:4 * j + 4])
    o["ln2g"] = np.ascontiguousarray(inp["ln2_g"][l].reshape(1, D))
    o["ln2b"] = np.ascontiguousarray(inp["ln2_b"][l].reshape(1, D))
    return o


INPUT_SHAPES = None


class Prog:
    def __init__(self, stage=99, dbg=False):
        self.stage = stage
        self.dbg = dbg
        self.nc = nc = bass.Bass("TRN2", target_bir_lowering=False)
        self.k = K(nc)
        self.inp = {}
        self.dbg_outs = {}
        self.pid = None
        self._psi = 0
        self._pidc = {}

    def din(self, name, shape, dt=F32):
        self.inp[name] = self.nc.dram_tensor(name, list(shape), dt, kind="ExternalInput").ap()
        return self.inp[name]

    def dout(self, name, shape, dt=F32):
        return self.nc.dram_tensor(name, list(shape), dt, kind="ExternalOutput").ap()

    def dscr(self, name, shape, dt=F32):
        return self.nc.dram_tensor(name, list(shape), dt).ap()

    def dump(self, name, bufs, ap, shape, dt=F32):
        if not self.dbg:
            return
        if not isinstance(bufs, (list, tuple)):
            bufs = [bufs]
        o = self.dout("dbg_" + name, shape, dt)
        self.k.dma("sync", o, ap, r=list(bufs), final=True)

    def mm(self, out, lhsT, rhs, start, stop, r, w):
        self.k.op("tensor", lambda e: e.matmul(out, lhsT=lhsT, rhs=rhs, start=start, stop=stop), r=r, w=w)

    def tr(self, out, in_, ident, r, w):
        self.k.op("tensor", lambda e: e.transpose(out=out, in_=in_, identity=ident), r=r, w=w)

    def act(self, out, in_, func, r, w, eng="scalar", **kw):
        self.k.op("scalar", lambda e: e.activation(out=out, in_=in_, func=func, **kw), r=r, w=w)

    def tt(self, out, in0, in1, op, r, w, eng="vector"):
        self.k.op(eng, lambda e: e.tensor_tensor(out=out, in0=in0, in1=in1, op=op), r=r, w=w)

    def ts(self, out, in0, s1, s2, op0, op1, r, w, eng="vector", **kw):
        if op1 is None:
            self.k.op(eng, lambda e: e.tensor_scalar(out=out, in0=in0, scalar1=s1, scalar2=None, op0=op0, **kw), r=r, w=w)
        else:
            self.k.op(eng, lambda e: e.tensor_scalar(out=out, in0=in0, scalar1=s1, scalar2=s2, op0=op0, op1=op1, **kw), r=r, w=w)

    def stt(self, out, in0, scalar, in1, op0, op1, r, w, eng="vector"):
        self.k.op(eng, lambda e: e.scalar_tensor_tensor(out=out, in0=in0, scalar=scalar, in1=in1, op0=op0, op1=op1), r=r, w=w)

    def rsqrt(self, out, in_, scale, r, w):
        self.act(out, in_, AF.Sqrt, r=list(r) + [self.eps_t], w=w, scale=scale, bias=self.eps_t.t[:, 0:1])
        self.k.op("vector", lambda e: e.reciprocal(out=out, in_=out), r=w, w=w)

    def cp(self, out, in_, r, w, eng="vector"):
        if eng == "scalar":
            self.k.op("scalar", lambda e: e.activation(out=out, in_=in_, func=AF.Copy), r=r, w=w)
        else:
            self.k.op(eng, lambda e: e.tensor_copy(out=out, in_=in_), r=r, w=w)

    def memset(self, ap, val, w, eng="vector"):
        self.k.op(eng, lambda e: e.memset(ap, val), w=w)

    def nps(self):
        b = self.PS[self._psi % len(self.PS)]
        self._psi += 1
        return b

    def load(self, name, shape, src_ap, dt=F32, eng="sync"):
        b = self.k.sb(name, shape, dt)
        self.k.dma(eng, b.t[:], src_ap, w=[b])
        return b

    def load_bf(self, name, shape, src_ap, conv_eng="vector"):
        b = self.k.sb(name, shape, BF16)
        a, n = shape[1], shape[2]
        if getattr(self, "_stg_mark", None) != self.k.sb_epoch or self._stg_n < n:
            self._stg_mark = self.k.sb_epoch
            self._stg = [self.k.sb(f"stg{i}", [128, max(n, 1024)], F32) for i in range(2)]
            self._stg_n = max(n, 1024)
            self._stg_i = 0
        for i in range(a):
            st = self._stg[self._stg_i % 2]
            self._stg_i += 1
            self.k.dma("sync", st.t[:, 0:n], src_ap[:, i, :], w=[st])
            self.cp(b.t[:, i, :], st.t[:, 0:n], r=[st], w=[b], eng=conv_eng)
        return b

    def dyn_dma(self, eng, out, mk_in, r=(), w=(), mult=SO):
        def fn(e):
            key = (eng, mult)
            if key not in self._pidc:
                if (eng, "pid") not in self._pidc:
                    self._pidc[(eng, "pid")] = e.partition_id()
                pid = self._pidc[(eng, "pid")]
                self._pidc[key] = e.snap((pid & 3) * mult, min_val=0, max_val=3 * mult)
            base = self._pidc[key]
            return e.dma_start(out=out, in_=mk_in(base))
        return self.k.custom_dma(eng, fn, r=r, w=w)

    def range_reduce(self, out, ang, tmp, r, w, eng="vector"):
        self.ts(tmp, ang, 1.0 / (2 * PI), MAGIC, ALU.mult, ALU.add, r=r, w=w, eng=eng)
        self.ts(tmp, tmp, -MAGIC, None, ALU.add, None, r=w, w=w, eng=eng)
        if eng == "vector":
            self.stt(out, tmp, -2 * PI, ang, ALU.mult, ALU.add, r=list(r) + list(w), w=w)
        else:
            self.ts(tmp, tmp, -2 * PI, None, ALU.mult, None, r=w, w=w, eng=eng)
            self.tt(out, tmp, ang, ALU.add, r=list(r) + list(w), w=w, eng=eng)
        self.ts(out, out, PI, -PI, ALU.min, ALU.max, r=w, w=w, eng=eng)


def phase01(P):
    k, nc = P.k, P.nc
    hc = P.hc
    xb = P.din("xb", [S, D])
    cvec = P.din("cvec", [128, 8])
    posf = P.din("posf", [1, S], I32)
    w_ada = P.din("w_ada", [D, 6 * D])
    b_ada = P.din("b_ada", [1, 6 * D])
    W1 = P.din("W1", [D, 768])
    wcq = P.din("wcq", [D, 256])
    qg = P.din("qg", [128, 2])
    kvg = P.din("kvg", [128, 1])
    invf = P.din("invf", [128, 1])
    sgn = P.din("sgn", [128, 1])
    P.PS = [k.ps(f"ps{i}", [128, 512], F32) for i in range(8)]
    P.ident_f = k.sb("ident_f", [128, 128], F32)
    P.memset(P.ident_f.t[:], 0.0, w=[P.ident_f], eng="gpsimd")
    k.op("gpsimd", lambda e: e.affine_select(out=P.ident_f.t[:], in_=P.ident_f.t[:], pattern=[[-1, 128]],
                                             compare_op=ALU.not_equal, fill=1.0, base=0, channel_multiplier=1),
         r=[P.ident_f], w=[P.ident_f])
    P.ident_b = k.sb("ident_b", [128, 128], BF16)
    P.cp(P.ident_b.t[:], P.ident_f.t[:], r=[P.ident_f], w=[P.ident_b])
    P.eps_t = k.sb("eps_t", [128, 1], F32)
    P.memset(P.eps_t.t[:], EPS, w=[P.eps_t])
    P.ones_b = k.sb("ones_b", [128, 128], BF16)
    P.memset(P.ones_b.t[:], 1.0, w=[P.ones_b])
    P.invf = P.load("invf", [128, 1], invf)
    P.sgn = P.load("sgn", [128, 1], sgn)
    P.qg = P.load("qg", [128, 2], qg)
    P.kvg = P.load("kvg", [128, 1], kvg)
    P.modR = k.sb("modR", [128, 4, D], F32)
    P.modT = k.sb("modT", [128, 2, 8], F32)
    P.a_tok = k.sb("a_tok", [128, 16, 512], BF16)
    P.a_l = [k.buf(f"a{t}") for t in range(16)]
    P.mark_attn = k.mark()
    P.ckvn = k.sb("ckvn", [128, S], BF16)
    P.krot = k.sb("krot", [128, S], BF16)
    P.hy_scr = P.dscr("hy_scr", [128, 3, S], BF16)
    P.cqn = k.sb("cqn", [128, 2, SO], BF16)
    P.cosO = k.sb("cosO", [128, SO], F32)
    P.sinO = k.sb("sinO", [128, SO], F32)
    ckvn_l = [k.buf(f"ckvn{t}") for t in range(16)]
    krot_l = [k.buf(f"krot{t}") for t in range(16)]
    hy_l = [k.buf(f"hy{t}") for t in range(16)]
    P.hyst = None
    cqn_l = [k.buf(f"cqn{t}") for t in range(4)]
    cs_l = [k.buf(f"cs{t}") for t in range(4)]
    P.ckvn_l, P.krot_l, P.hy_l, P.cqn_l, P.cs_l = ckvn_l, krot_l, hy_l, cqn_l, cs_l
    mark = k.mark()
    cT = P.load("cT", [128, 8], cvec)
    sc = k.sb("sc", [128, 8], F32)
    P.act(sc.t[:], cT.t[:], AF.Silu, r=[cT], w=[sc])
    sc_rep = k.sb("sc_rep", [128, 8, 128], F32)
    P.cp(sc_rep.t[:], sc.t[:].unsqueeze(2).to_broadcast([128, 8, 128]), r=[sc], w=[sc_rep])
    bada = P.load("bada", [128, 6 * D], b_ada.to_broadcast([128, 6 * D]))
    modA = k.sb("modA", [128, 2 * D], F32)
    wa = [k.sb(f"wa{i}", [128, 8, 512], F32) for i in range(2)]
    w_ada_v = w_ada.rearrange("(k p) n -> p k n", p=128)
    for cg in range(12):
        wt = wa[cg % 2]
        k.dma("sync", wt.t[:], w_ada_v[:, :, cg * 512:(cg + 1) * 512], w=[wt])
        ps = P.nps()
        for kc in range(8):
            P.mm(ps.t[:], sc_rep.t[:, kc, :], wt.t[:, kc, :], kc == 0, kc == 7, r=[sc_rep, wt], w=[ps])
        m, half = cg // 2, cg % 2
        if m < 2:
            dst = modA.t[:, m * D + half * 512: m * D + half * 512 + 512]
            dbuf = modA
        else:
            dst = P.modR.t[:, m - 2, half * 512: half * 512 + 512]
            dbuf = P.modR
        P.tt(dst, ps.t[:], bada.t[:, cg * 512:(cg + 1) * 512], ALU.add, r=[ps, bada], w=[dbuf])
    P.ts(modA.t[:, D:2 * D], modA.t[:, D:2 * D], 1.0, None, ALU.add, None, r=[modA], w=[modA])
    P.ts(P.modR.t[:, 2, :], P.modR.t[:, 2, :], 1.0, None, ALU.add, None, r=[P.modR], w=[P.modR])
    for m in range(2):
        for blk in range(8):
            ps = P.nps()
            P.tr(ps.t[:, 0:128], modA.t[:, m * D + blk * 128: m * D + (blk + 1) * 128], P.ident_f.t[:], r=[modA, P.ident_f], w=[ps])
            P.cp(P.modT.t[:, m, blk:blk + 1], ps.t[:, 0:1], r=[ps], w=[P.modT])
    if P.dbg:
        P.dump("sc", sc, sc.t[:], [128, 8])
        P.dump("screp", sc_rep, sc_rep.t[:, :, 0:2], [128, 8, 2])
        P.dump("modA", modA, modA.t[0:1, :], [1, 2 * D])
        P.dump("modR", P.modR, P.modR.t[0:1, :, :], [1, 4, D])
        P.dump("modT", P.modT, P.modT.t[:], [128, 2, 8])
    k.release(mark)
    if P.stage <= 0:
        return
    mark = k.mark()
    W1b = P.load_bf("W1b", [128, 8, 768], W1.rearrange("(k p) n -> p k n", p=128))
    wcqb = P.load_bf("wcqb", [128, 8, 256], wcq.rearrange("(k p) n -> p k n", p=128))
    xt = [k.sb(f"xt{i}", [128, 4, D], F32) for i in range(2)]
    uT = [k.sb(f"uT{i}", [128, 8, 512], BF16) for i in range(2)]
    posi = [k.sb(f"posi{i}", [128, 512], I32) for i in range(2)]
    ang = k.sb("ang", [128, 512], F32)
    ang2 = k.sb("ang2", [128, 512], F32)
    tmp = k.sb("tmp", [128, 512], F32)
    rr = k.sb("rr", [128, 512], F32)
    cosv = k.sb("cosv", [128, 512], F32)
    sinx = k.sb("sinx", [128, 512], F32)
    sq = [k.sb(f"sq{i}", [128, 512], BF16) for i in range(2)]
    rstd = k.sb("rstd", [128, 512], F32)
    t1 = k.sb("t1", [128, 512], F32)
    t2 = k.sb("t2", [128, 512], F32)
    hyst = [k.sb(f"hyst{i}", [128, 3, 512], BF16) for i in range(2)]

    def make_uT(i, xtile, utile):
        for kc in range(8):
            ps = P.nps()
            for s in range(4):
                P.tr(ps.t[:, s * 128:(s + 1) * 128], xtile.t[:, s, kc * 128:(kc + 1) * 128], P.ident_f.t[:],
                     r=[xtile, P.ident_f], w=[ps])
            P.act(utile.t[:, kc, :], ps.t[:], AF.Identity, r=[ps, P.modT], w=[utile],
                  scale=P.modT.t[:, 1, kc:kc + 1], bias=P.modT.t[:, 0, kc:kc + 1])

    def rope_tables(pos_tile, cos_out, sin_out, cos_buf, sin_buf):
        P.cp(ang.t[:], pos_tile.t[:], r=[pos_tile], w=[ang])
        P.ts(ang.t[:], ang.t[:], P.invf.t[:, 0:1], None, ALU.mult, None, r=[ang, P.invf], w=[ang])
        P.range_reduce(rr.t[:], ang.t[:], tmp.t[:], r=[ang], w=[tmp, rr])
        P.act(sin_out, rr.t[:], AF.Sin, r=[rr, P.sgn], w=[sin_buf], scale=P.sgn.t[:, 0:1])
        P.ts(ang2.t[:], ang.t[:], PI / 2, None, ALU.add, None, r=[ang], w=[ang2])
        P.range_reduce(rr.t[:], ang2.t[:], tmp.t[:], r=[ang2], w=[tmp, rr])
        P.act(cos_out, rr.t[:], AF.Sin, r=[rr], w=[cos_buf])

    def rms_T(ps_list, g_ap_list, out_aps, out_bufs, nfeat):
        ss = P.nps()
        for i, ps in enumerate(ps_list):
            P.act(sq[i].t[:], ps.t[:], AF.Square, r=[ps], w=[sq[i]])
            P.mm(ss.t[:], P.ones_b.t[:], sq[i].t[:], i == 0, i == len(ps_list) - 1, r=[P.ones_b, sq[i]], w=[ss])
        P.rsqrt(rstd.t[:], ss.t[:], 1.0 / nfeat, r=[ss], w=[rstd])
        for i, ps in enumerate(ps_list):
            P.stt(out_aps[i], ps.t[:], g_ap_list[i], rstd.t[:], ALU.mult, ALU.mult, r=[ps, rstd], w=[out_bufs[i]])

    xv = xb.rearrange("(t s p) d -> t p s d", p=128, s=4)
    for tt_ in range(16):
        xtile, utile, ptile = xt[tt_ % 2], uT[tt_ % 2], posi[tt_ % 2]
        k.dma("sync", xtile.t[:], xv[tt_], w=[xtile])
        k.dma("sync", ptile.t[:], posf[0:1, tt_ * 512:(tt_ + 1) * 512].to_broadcast([128, 512]), w=[ptile])
        make_uT(tt_, xtile, utile)
        pss = []
        for oc in range(6):
            ps = P.nps()
            for kc in range(8):
                P.mm(ps.t[:], W1b.t[:, kc, oc * 128:(oc + 1) * 128], utile.t[:, kc, :], kc == 0, kc == 7, r=[W1b, utile], w=[ps])
            pss.append(ps)
        sl = slice(tt_ * 512, (tt_ + 1) * 512)
        hs = hyst[tt_ % 2]
        for g in range(3):
            P.cp(hs.t[:, g, :], pss[3 + g].t[:], r=[pss[3 + g]], w=[hs], eng="scalar")
        k.dma("sync", P.hy_scr[:, :, sl], hs.t[:], r=[hs], w=[hy_l[tt_]])
        rope_tables(ptile, cosv.t[:], sinx.t[:], cosv, sinx)
        P.tt(t1.t[:], pss[1].t[:], cosv.t[:], ALU.mult, r=[pss[1], cosv], w=[t1])
        P.tt(t2.t[:], pss[2].t[:], sinx.t[:], ALU.mult, r=[pss[2], sinx], w=[t2])
        P.tt(P.krot.t[:, sl], t1.t[:], t2.t[:], ALU.add, r=[t1, t2], w=[krot_l[tt_]], eng="gpsimd")
        rms_T([pss[0]], [P.kvg.t[:, 0:1]], [P.ckvn.t[:, sl]], [ckvn_l[tt_]], 128)
    for tt_ in range(4):
        xtile, utile, ptile = xt[tt_ % 2], uT[tt_ % 2], posi[tt_ % 2]
        P.dyn_dma("sync", xtile.t[:], lambda base, tt_=tt_: xb[bass.ds(base, SO), :][tt_ * 512:(tt_ + 1) * 512, :].rearrange("(s p) d -> p s d", p=128), w=[xtile])
        P.dyn_dma("sync", ptile.t[:], lambda base, tt_=tt_: posf[0:1, bass.ds(base, SO)][:, tt_ * 512:(tt_ + 1) * 512].to_broadcast([128, 512]), w=[ptile])
        make_uT(tt_, xtile, utile)
        pss = []
        for oc in range(2):
            ps = P.nps()
            for kc in range(8):
                P.mm(ps.t[:], wcqb.t[:, kc, oc * 128:(oc + 1) * 128], utile.t[:, kc, :], kc == 0, kc == 7, r=[wcqb, utile], w=[ps])
            pss.append(ps)
        sl = slice(tt_ * 512, (tt_ + 1) * 512)
        rope_tables(ptile, P.cosO.t[:, sl], P.sinO.t[:, sl], cs_l[tt_], cs_l[tt_])
        rms_T(pss, [P.qg.t[:, 0:1], P.qg.t[:, 1:2]], [P.cqn.t[:, 0, sl], P.cqn.t[:, 1, sl]], [cqn_l[tt_], cqn_l[tt_]], 256)
    k.release(mark)
    if P.dbg and P.stage == 1:
        P.dump("ckvn", ckvn_l, P.ckvn.t[:], [128, S], BF16)
        P.dump("krot", krot_l, P.krot.t[:], [128, S], BF16)
        P.dump("cqn", cqn_l, P.cqn.t[:], [128, 2, SO], BF16)
        P.dump("cosO", cs_l, P.cosO.t[:], [128, SO])
        P.dump("sinO", cs_l, P.sinO.t[:], [128, SO])
        o = P.dout("dbg_hyT", [128, 3, S], BF16)
        k.dma("sync", o, P.hy_scr, r=hy_l, final=True)


def phase2(P):
    k = P.k
    wq = P.din("wq", [256, H * 2 * 128])
    wk = P.din("wk", [128, H * 128])
    wv = P.din("wv", [128, H * 64])
    mark = k.mark()
    wqb = P.load_bf("wqb", [128, 2, H * 2 * 128], wq.rearrange("(k p) n -> p k n", p=128))
    wkb = P.load_bf("wkb", [128, 1, H * 128], wk.rearrange("(k p) n -> p k n", p=128))
    wvb = P.load_bf("wvb", [128, 1, H * 64], wv.rearrange("(k p) n -> p k n", p=128))
    NS = 2
    KT = [k.sb(f"KT{i}", [128, S], BF16) for i in range(NS)]
    VX = [k.sb(f"VX{i}", [128, 64, 65], BF16) for i in range(NS)]
    QT = [k.sb(f"QT{i}", [128, SO], BF16) for i in range(NS)]
    for v in VX:
        P.memset(v.t[:, :, 64:65], 1.0, w=[v])
    PT = [k.sb(f"PT{i}", [128, 512], BF16) for i in range(4)]
    t1 = k.sb("at1", [128, 512], F32)
    t2 = k.sb("at2", [128, 512], F32)
    rc = k.sb("rc", [128, 4], F32)
    allps = P.PS
    P.PS = allps[0:4]
    acc = allps[4:8]
    pti = 0
    for h in range(H):
        kt_, vx, qt_ = KT[h % NS], VX[h % NS], QT[h % NS]
        for tg in range(16):
            sl = slice(tg * 512, (tg + 1) * 512)
            ps = P.nps()
            P.mm(ps.t[:], wkb.t[:, 0, h * 128:(h + 1) * 128], P.ckvn.t[:, sl], True, True, r=[wkb, P.ckvn_l[tg]], w=[ps])
            P.tt(kt_.t[:, sl], ps.t[:], P.krot.t[:, sl], ALU.add, r=[ps, P.krot_l[tg]], w=[kt_])
        for g8 in range(8):
            ps = P.nps()
            for i in range(8):
                kt = g8 * 8 + i
                P.mm(ps.t[:, i * 64:(i + 1) * 64], P.ckvn.t[:, kt * 128:(kt + 1) * 128], wvb.t[:, 0, h * 64:(h + 1) * 64], True, True,
                     r=[wvb, P.ckvn_l[kt // 4]], w=[ps])
            P.cp(vx.t[:, g8 * 8:(g8 + 1) * 8, 0:64], ps.t[:].rearrange("p (i d) -> p i d", i=8), r=[ps], w=[vx], eng="scalar")
        for tg in range(4):
            sl = slice(tg * 512, (tg + 1) * 512)
            psR, psP = P.nps(), P.nps()
            for kc in range(2):
                P.mm(psR.t[:], wqb.t[:, kc, (h * 2) * 128:(h * 2 + 1) * 128], P.cqn.t[:, kc, sl], kc == 0, kc == 1, r=[wqb, P.cqn_l[tg]], w=[psR])
            for kc in range(2):
                P.mm(psP.t[:], wqb.t[:, kc, (h * 2 + 1) * 128:(h * 2 + 2) * 128], P.cqn.t[:, kc, sl], kc == 0, kc == 1, r=[wqb, P.cqn_l[tg]], w=[psP])
            P.tt(t1.t[:], psR.t[:], P.cosO.t[:, sl], ALU.mult, r=[psR, P.cs_l[tg]], w=[t1])
            P.tt(t2.t[:], psP.t[:], P.sinO.t[:, sl], ALU.mult, r=[psP, P.cs_l[tg]], w=[t2])
            P.tt(qt_.t[:, sl], t1.t[:], t2.t[:], ALU.add, r=[t1, t2], w=[qt_], eng="gpsimd")
        LOOK = 2
        iters = [(qg, kt) for qg in range(4) for kt in range(64)]
        pend = []

        def emit_score(qg, kt):
            nonlocal pti
            ps = P.nps()
            P.mm(ps.t[:], kt_.t[:, kt * 128:(kt + 1) * 128], qt_.t[:, qg * 512:(qg + 1) * 512], True, True, r=[kt_, qt_], w=[ps])
            pt = PT[pti % len(PT)]
            pti += 1
            P.act(pt.t[:], ps.t[:], AF.Exp, r=[ps], w=[pt], scale=SCALE)
            return pt

        def emit_pv(qg, kt, pt):
            for q4 in range(4):
                P.mm(acc[q4].t[:, 0:65], pt.t[:, q4 * 128:(q4 + 1) * 128], vx.t[:, kt, :], kt == 0, kt == 63, r=[pt, vx], w=[acc[q4]])
            if kt == 63:
                for q4 in range(4):
                    tile_i = qg * 4 + q4
                    k.op("vector", lambda e, q4=q4: e.reciprocal(out=rc.t[:, q4:q4 + 1], in_=acc[q4].t[:, 64:65]), r=[acc[q4]], w=[rc])
                    P.ts(P.a_tok.t[:, tile_i, h * 64:(h + 1) * 64], acc[q4].t[:, 0:64], rc.t[:, q4:q4 + 1], None, ALU.mult, None,
                         r=[acc[q4], rc], w=[P.a_l[tile_i]])

        for idx, (qg, kt) in enumerate(iters):
            pend.append((qg, kt, emit_score(qg, kt)))
            if len(pend) > LOOK:
                emit_pv(*pend.pop(0))
        while pend:
            emit_pv(*pend.pop(0))
    P.PS = allps
    k.release(P.mark_attn)
    if P.dbg and P.stage == 2:
        P.dump("a_tok", P.a_l, P.a_tok.t[:], [128, 16, 512], BF16)


def build_all(P):
    phase01(P)
    if P.stage <= 1:
        return
    if os.environ.get("SKIP2", "0") != "1":
        phase2(P)
    else:
        P.k.release(P.mark_attn)
    if P.stage <= 2:
        return
    phase3(P)
    if P.stage <= 3:
        return
    phase4(P)
    if P.stage <= 4:
        return
    phase5(P)
    phase6(P)


def phase3(P):
    k = P.k
    L1 = float(S - 1)
    cw = P.din("cw", [128, 9]); cb = P.din("cb", [128, 3])
    featsT = P.din("featsT", [33, NFFT])
    fw1 = P.din("fw1", [33, 64]); fb1 = P.din("fb1", [64, 1]); ffreq = P.din("ffreq", [64, 1])
    fw2 = P.din("fw2", [64, 64]); fb2 = P.din("fb2", [64, 1])
    fw3 = P.din("fw3", [64, 512]); fb3 = P.din("fb3", [1, 512]); fdec = P.din("fdec", [1, 512])
    hbias = P.din("hbias", [1, 256])
    e1s = P.din("e1s", [128, 1]); n2row = P.din("n2row", [128, 128])
    cn = {n: P.din(n, shp) for n, shp in [("F1", [128, 256]), ("C2", [128, 128]), ("S2", [128, 128]), ("S2n", [128, 128]),
                                           ("G1", [128, 256]), ("G2", [128, 256]), ("TC", [128, 128]), ("TS", [128, 128]),
                                           ("TSn", [128, 128]), ("CI", [128, 64]), ("SIn", [128, 64])]}
    P.hy_bounce = [P.dscr(f"hy_bounce{g}", [32, S], F32) for g in range(4)]
    P.hyb_l = [k.buf(f"hy_bounce{g}") for g in range(4)]
    P.hy_all = [P.dscr(f"hy_all{g}", [4 * 32, S], F32) for g in range(4)]
    P.hya_l = [k.buf(f"hy_all{g}") for g in range(4)]
    L_scr = [P.dscr(f"L_scr{g}", [64, 128, 128], BF16) for g in range(3)]
    L_lab = [k.buf(f"L_scr{g}") for g in range(3)]
    z2_scr = P.dscr("z2_scr", [64, 128, 128], BF16)
    z2_lab = [k.buf(f"z2_{g}") for g in range(4)]
    mark = k.mark()
    def ldc_bf(name, shape):
        st = P.load(name + "_f", shape, cn[name])
        b = k.sb(name + "_b", shape, BF16)
        P.cp(b.t[:], st.t[:], r=[st], w=[b])
        return b
    TC = P.load("TC", [128, 128], cn["TC"]); TS = P.load("TS", [128, 128], cn["TS"]); TSn = P.load("TSn", [128, 128], cn["TSn"])
    mk2 = k.mark()
    F1b = k.sb("F1b", [128, 256], BF16); C2b = k.sb("C2b", [128, 128], BF16); S2b = k.sb("S2b", [128, 128], BF16)
    S2nb = k.sb("S2nb", [128, 128], BF16); G1b = k.sb("G1b", [128, 256], BF16); G2b = k.sb("G2b", [128, 256], BF16)
    CIb = k.sb("CIb", [128, 64], BF16); SInb = k.sb("SInb", [128, 64], BF16)
    stg = k.sb("cstg", [128, 256], F32)
    for nm, b, w_ in [("F1", F1b, 256), ("C2", C2b, 128), ("S2", S2b, 128), ("S2n", S2nb, 128), ("G1", G1b, 256), ("G2", G2b, 256),
                      ("CI", CIb, 64), ("SIn", SInb, 64)]:
        k.dma("sync", stg.t[:, 0:w_], cn[nm], w=[stg])
        P.cp(b.t[:], stg.t[:, 0:w_], r=[stg], w=[b])
    cwt = P.load("cwt", [128, 9], cw); cbt = P.load("cbt", [128, 3], cb)
    e1st = P.load("e1st", [128, 1], e1s); n2r = P.load("n2r", [128, 128], n2row)
    b3bc = P.load("b3bc", [128, 512], fb3.to_broadcast([128, 512]))
    decbc = P.load("decbc", [128, 512], fdec.to_broadcast([128, 512]))
    dneg = k.sb("dneg", [128, 512], F32)
    P.ts(dneg.t[:], decbc.t[:], -1.0, None, ALU.mult, None, r=[decbc], w=[dneg])
    P.tt(decbc.t[:], decbc.t[:], dneg.t[:], ALU.max, r=[decbc, dneg], w=[decbc])
    hbbc = P.load("hbbc", [128, 256], hbias.to_broadcast([128, 256]))
    w3st = P.load("w3st", [64, 512], fw3)
    w3b = k.sb("w3b", [64, 512], BF16)
    P.cp(w3b.t[:], w3st.t[:], r=[w3st], w=[w3b])
    h2T = k.sb("h2T", [64, NFFT], BF16)
    mk3 = k.mark()
    w1t = P.load("w1t", [33, 64], fw1); w2t = P.load("w2t", [64, 64], fw2)
    b1t = P.load("b1t", [64, 1], fb1); b2t = P.load("b2t", [64, 1], fb2); frt = P.load("frt", [64, 1], ffreq)
    fb1f = k.sb("fb1f", [64, 1], F32); fb2f = k.sb("fb2f", [64, 1], F32)
    P.tt(fb1f.t[:], b1t.t[:], frt.t[:], ALU.mult, r=[b1t, frt], w=[fb1f])
    P.tt(fb2f.t[:], b2t.t[:], frt.t[:], ALU.mult, r=[b2t, frt], w=[fb2f])
    fe = [k.sb(f"fe{i}", [33, 2048], F32) for i in range(2)]
    arg = k.sb("farg", [64, 512], F32); ftmp = k.sb("ftmp", [64, 512], F32); frr = k.sb("frr", [64, 512], F32)
    h1 = k.sb("fh1", [64, 512], F32)
    arg2 = k.sb("farg2", [64, 512], F32); ftmp2 = k.sb("ftmp2", [64, 512], F32); frr2 = k.sb("frr2", [64, 512], F32)
    for ch in range(8):
        f = fe[ch % 2]
        k.dma("sync", f.t[:], featsT[:, ch * 2048:(ch + 1) * 2048], w=[f])
        for c5 in range(4):
            col = ch * 2048 + c5 * 512
            ps = P.nps()
            P.mm(ps.t[0:64, :], w1t.t[:, :], f.t[:, c5 * 512:(c5 + 1) * 512], True, True, r=[w1t, f], w=[ps])
            P.ts(arg.t[:], ps.t[0:64, :], frt.t[:, 0:1], fb1f.t[:, 0:1], ALU.mult, ALU.add, r=[ps, frt, fb1f], w=[arg])
            P.range_reduce(frr.t[:], arg.t[:], ftmp.t[:], r=[arg], w=[ftmp, frr])
            P.act(h1.t[:], frr.t[:], AF.Sin, r=[frr], w=[h1])
            ps2 = P.nps()
            P.mm(ps2.t[0:64, :], w2t.t[:, :], h1.t[:, :], True, True, r=[w2t, h1], w=[ps2])
            P.ts(arg2.t[:], ps2.t[0:64, :], frt.t[:, 0:1], fb2f.t[:, 0:1], ALU.mult, ALU.add, r=[ps2, frt, fb2f], w=[arg2])
            P.range_reduce(frr2.t[:], arg2.t[:], ftmp2.t[:], r=[arg2], w=[ftmp2, frr2])
            P.act(h2T.t[:, col:col + 512], frr2.t[:], AF.Sin, r=[frr2], w=[h2T])
    k.release(mk3)
    mk3 = k.mark()
    raw = k.sb("raw", [128, S + 2], BF16)
    uc = k.sb("uc", [128, S], BF16)
    accb = k.sb("accb", [128, 2048], F32)
    Lst = k.sb("Lst", [64, 128, 128], BF16)
    for g in range(3):
        P.memset(raw.t[:, 0:1], 0.0, w=[raw])
        P.memset(raw.t[:, S + 1:S + 2], 0.0, w=[raw])
        k.dma("sync", raw.t[:, 1:S + 1], P.hy_scr[:, g, :], r=P.hy_l, w=[raw])
        for c4 in range(4):
            c0 = c4 * 2048
            P.ts(accb.t[:], raw.t[:, c0 + 1:c0 + 2049], cwt.t[:, g * 3 + 1:g * 3 + 2], cbt.t[:, g:g + 1], ALU.mult, ALU.add, r=[raw, cwt, cbt], w=[accb])
            P.stt(accb.t[:], raw.t[:, c0:c0 + 2048], cwt.t[:, g * 3:g * 3 + 1], accb.t[:], ALU.mult, ALU.add, r=[raw, cwt, accb], w=[accb])
            P.stt(uc.t[:, c0:c0 + 2048], raw.t[:, c0 + 2:c0 + 2050], cwt.t[:, g * 3 + 2:g * 3 + 3], accb.t[:], ALU.mult, ALU.add, r=[raw, cwt, accb], w=[uc])
        ucv = uc.t[:, :].rearrange("p (a b) -> p b a", b=128)
        for b8 in range(16):
            ps = P.nps()
            psb = ps.t[:].bitcast(BF16)
            for i in range(8):
                n2 = b8 * 8 + i
                P.tr(psb[0:64, i * 128:(i + 1) * 128], ucv[:, n2, :], P.ident_b.t[:], r=[uc, P.ident_b], w=[ps])
            P.cp(Lst.t[:, :, b8 * 8:(b8 + 1) * 8].rearrange("p c i -> p i c"), psb[0:64, :].rearrange("p (i c) -> p i c", c=128),
                 r=[ps], w=[Lst], eng=("scalar" if b8 % 2 else "vector"))
        k.dma("sync", L_scr[g], Lst.t[:], r=[Lst], w=[L_lab[g]])
    k.release(mk3)
    hraw = k.sb("hraw", [128, 32, 128], F32)
    E2 = k.sb("E2", [128, 32, 128], F32)
    hL = k.sb("hL", [128, 32, 128], BF16)
    absd = k.sb("absd", [128, 32], F32); dsel = k.sb("dsel", [128, 32], F32); E1 = k.sb("E1", [128, 32], F32)
    Hre = k.sb("Hre", [128, 32, 128], BF16); Him = k.sb("Him", [128, 32, 128], BF16)
    Are = k.sb("Are", [128, 32, 128], BF16); Aim = k.sb("Aim", [128, 32, 128], BF16)
    Yre = k.sb("Yre", [128, 32, 128], BF16); Yim = k.sb("Yim", [128, 32, 128], BF16)
    zg = k.sb("zg", [64, 32, 128], BF16); xg = k.sb("xg", [64, 32, 128], BF16)
    z2g = hL; outg = hraw
    MS = [[k.sb(f"m{i}_{q}", [128, 512], F32) for i in range(4)] for q in range(2)]
    msi = [0]

    def next_ms():
        msi[0] += 1
        return MS[msi[0] % len(MS)]
    h2v = h2T.t[:, :].rearrange("p (a b) -> p b a", b=128)

    def twiddle(psA, dre, dim, c0, inverse):
        m1, m2, m3, m4 = next_ms()
        v = psA.t[:].rearrange("p (c r k) -> p c r k", c=2, r=2)
        are, aim = v[:, :, 0, :], v[:, :, 1, :]
        tc = TC.t[:].unsqueeze(1).to_broadcast([128, 2, 128])
        tsn = TSn.t[:].unsqueeze(1).to_broadcast([128, 2, 128])
        tsp = TS.t[:].unsqueeze(1).to_broadcast([128, 2, 128])
        a, b, c_, d = (m1.t[:, 0:256].rearrange("p (c k) -> p c k", c=2), m2.t[:, 0:256].rearrange("p (c k) -> p c k", c=2),
                       m3.t[:, 0:256].rearrange("p (c k) -> p c k", c=2), m4.t[:, 0:256].rearrange("p (c k) -> p c k", c=2))
        P.tt(a, are, tc, ALU.mult, r=[psA, TC], w=[m1])
        P.tt(b, aim, tsn if inverse else tsp, ALU.mult, r=[psA, TS, TSn], w=[m2])
        P.tt(dre.t[:, c0:c0 + 2, :], a, b, ALU.add, r=[m1, m2], w=[dre], eng="gpsimd")
        P.tt(c_, aim, tc, ALU.mult, r=[psA, TC], w=[m3])
        P.tt(d, are, tsp if inverse else tsn, ALU.mult, r=[psA, TS, TSn], w=[m4])
        P.tt(dim.t[:, c0:c0 + 2, :], c_, d, ALU.add, r=[m3, m4], w=[dim], eng="gpsimd")

    def fwd_stage1(src, Kn):
        for c2 in range(16):
            ps = P.nps()
            for i in range(2):
                c = c2 * 2 + i
                P.mm(ps.t[:, i * 256:(i + 1) * 256], src.t[0:Kn, c, :], F1b.t[0:Kn, :], True, True, r=[src, F1b], w=[ps])
            twiddle(ps, Are, Aim, c2 * 2, False)

    for order in range(2):
        for grp in range(4):
            col0 = (order * 4 + grp) * 64
            P.cp(absd.t[0:64, :], decbc.t[0:64, col0:col0 + 32], r=[decbc], w=[absd])
            P.cp(absd.t[64:128, :], decbc.t[64:128, col0 + 32:col0 + 64], r=[decbc], w=[absd])
            P.ts(dsel.t[0:64, :], absd.t[0:64, :], -1.0 / L1, None, ALU.mult, None, r=[absd], w=[dsel])
            P.ts(dsel.t[64:128, :], absd.t[64:128, :], 1.0 / L1, None, ALU.mult, None, r=[absd], w=[dsel])
            P.act(E1.t[:], absd.t[:], AF.Exp, r=[absd, e1st], w=[E1], scale=e1st.t[:, 0:1])
            P.tt(E2.t[:], n2r.t[:].unsqueeze(1).to_broadcast([128, 32, 128]), dsel.t[:].unsqueeze(2).to_broadcast([128, 32, 128]),
                 ALU.mult, r=[n2r, dsel], w=[E2], eng="gpsimd")
            P.act(E2.t[:], E2.t[:], AF.Exp, r=[E2], w=[E2])
            for b8 in range(16):
                ps = P.nps()
                for i in range(8):
                    n2 = b8 * 8 + i
                    P.mm(ps.t[:, i * 64:(i + 1) * 64], h2v[:, n2, :], w3b.t[:, col0:col0 + 64], True, True, r=[h2T, w3b], w=[ps])
                pv = ps.t[:].rearrange("p (i c) -> p c i", c=64)
                P.tt(hraw.t[0:64, :, b8 * 8:(b8 + 1) * 8], pv[0:64, 0:32, :],
                     b3bc.t[0:64, col0:col0 + 32].unsqueeze(2).to_broadcast([64, 32, 8]), ALU.add, r=[ps, b3bc], w=[hraw])
                P.tt(hraw.t[64:128, :, b8 * 8:(b8 + 1) * 8], pv[64:128, 32:64, :],
                     b3bc.t[64:128, col0 + 32:col0 + 64].unsqueeze(2).to_broadcast([64, 32, 8]), ALU.add, r=[ps, b3bc], w=[hraw])
            P.tt(hraw.t[:], hraw.t[:], E1.t[:].unsqueeze(2).to_broadcast([128, 32, 128]), ALU.mult, r=[hraw, E1], w=[hraw])
            P.tt(hL.t[:], hraw.t[:], E2.t[:], ALU.mult, r=[hraw, E2], w=[hL], eng="gpsimd")
            P.memset(hL.t[64:65, :, 0:1], 0.0, w=[hL])
            fwd_stage1(hL, 128)
            for c8 in range(8):
                sl = slice(c8 * 4, c8 * 4 + 4)
                pre, pim = P.nps(), P.nps()
                ar = Are.t[:, sl, :].rearrange("p c k -> p (c k)"); ai = Aim.t[:, sl, :].rearrange("p c k -> p (c k)")
                P.mm(pre.t[:], C2b.t[:], ar, True, False, r=[C2b, Are], w=[pre])
                P.mm(pre.t[:], S2b.t[:], ai, False, True, r=[S2b, Aim], w=[pre])
                P.mm(pim.t[:], C2b.t[:], ai, True, False, r=[C2b, Aim], w=[pim])
                P.mm(pim.t[:], S2nb.t[:], ar, False, True, r=[S2nb, Are], w=[pim])
                P.cp(Hre.t[:, sl, :].rearrange("p c k -> p (c k)"), pre.t[:], r=[pre], w=[Hre], eng="scalar")
                P.cp(Him.t[:, sl, :].rearrange("p c k -> p (c k)"), pim.t[:], r=[pim], w=[Him], eng="scalar")
            src_ap = (L_scr[0] if order == 0 else z2_scr)[:, grp * 32:(grp + 1) * 32, :]
            k.dma("sync", zg.t[:], src_ap, r=([L_lab[0]] if order == 0 else [z2_lab[grp]]), w=[zg])
            k.dma("sync", xg.t[:], L_scr[1 + order][:, grp * 32:(grp + 1) * 32, :], r=[L_lab[1 + order]], w=[xg])
            fwd_stage1(zg, 64)
            for c8 in range(8):
                sl = slice(c8 * 4, c8 * 4 + 4)
                pre, pim = P.nps(), P.nps()
                ar = Are.t[:, sl, :].rearrange("p c k -> p (c k)"); ai = Aim.t[:, sl, :].rearrange("p c k -> p (c k)")
                P.mm(pre.t[:], C2b.t[:], ar, True, False, r=[C2b, Are], w=[pre])
                P.mm(pre.t[:], S2b.t[:], ai, False, True, r=[S2b, Aim], w=[pre])
                P.mm(pim.t[:], C2b.t[:], ai, True, False, r=[C2b, Aim], w=[pim])
                P.mm(pim.t[:], S2nb.t[:], ar, False, True, r=[S2nb, Are], w=[pim])
                hr = Hre.t[:, sl, :].rearrange("p c k -> p (c k)"); hi = Him.t[:, sl, :].rearrange("p c k -> p (c k)")
                m1, m2, m3, m4 = next_ms()
                P.tt(m1.t[:], pre.t[:], hr, ALU.mult, r=[pre, Hre], w=[m1])
                P.tt(m2.t[:], pim.t[:], hi, ALU.mult, r=[pim, Him], w=[m2])
                P.tt(Yre.t[:, sl, :].rearrange("p c k -> p (c k)"), m1.t[:], m2.t[:], ALU.subtract, r=[m1, m2], w=[Yre], eng="gpsimd")
                P.tt(m3.t[:], pre.t[:], hi, ALU.mult, r=[pre, Him], w=[m3])
                P.tt(m4.t[:], pim.t[:], hr, ALU.mult, r=[pim, Hre], w=[m4])
                P.tt(Yim.t[:, sl, :].rearrange("p c k -> p (c k)"), m3.t[:], m4.t[:], ALU.add, r=[m3, m4], w=[Yim], eng="gpsimd")
            for c2 in range(16):
                ps = P.nps()
                for i in range(2):
                    c = c2 * 2 + i
                    P.mm(ps.t[:, i * 256:(i + 1) * 256], Yre.t[:, c, :], G1b.t[:], True, False, r=[Yre, G1b], w=[ps])
                    P.mm(ps.t[:, i * 256:(i + 1) * 256], Yim.t[:, c, :], G2b.t[:], False, True, r=[Yim, G2b], w=[ps])
                twiddle(ps, Are, Aim, c2 * 2, True)
            hb0 = order * 128 + grp * 32
            for c8 in range(8):
                sl = slice(c8 * 4, c8 * 4 + 4)
                py = P.nps()
                br = Are.t[:, sl, :].rearrange("p c k -> p (c k)"); bi = Aim.t[:, sl, :].rearrange("p c k -> p (c k)")
                P.mm(py.t[0:64, :], CIb.t[:], br, True, False, r=[CIb, Are], w=[py])
                P.mm(py.t[0:64, :], SInb.t[:], bi, False, True, r=[SInb, Aim], w=[py])
                m1 = next_ms()[0]
                t_ = m1.t[0:64, :].rearrange("p (c k) -> p c k", c=4)
                P.tt(t_, zg.t[:, sl, :], hbbc.t[0:64, hb0 + c8 * 4:hb0 + c8 * 4 + 4].unsqueeze(2).to_broadcast([64, 4, 128]), ALU.mult,
                     r=[zg, hbbc], w=[m1])
                P.tt(m1.t[0:64, :], m1.t[0:64, :], py.t[0:64, :], ALU.add, r=[m1, py], w=[m1])
                if order == 0:
                    P.tt(z2g.t[0:64, sl, :], t_, xg.t[:, sl, :], ALU.mult, r=[m1, xg], w=[z2g], eng="gpsimd")
                else:
                    P.tt(outg.t[0:64, sl, :], t_, xg.t[:, sl, :], ALU.mult, r=[m1, xg], w=[outg], eng="gpsimd")
            if order == 0:
                k.dma("sync", z2_scr[:, grp * 32:(grp + 1) * 32, :], z2g.t[0:64, :, :], r=[z2g], w=[z2_lab[grp]])
            else:
                k.dma("sync", P.hy_bounce[grp].rearrange("c (a b) -> a c b", b=128), outg.t[0:64, :, :], r=[outg], w=[P.hyb_l[grp]])
                k.collective("AllGather", ALU.bypass, [[0, 1, 2, 3], [4, 5, 6, 7]], ins=[P.hy_bounce[grp]], outs=[P.hy_all[grp]],
                             r=[P.hyb_l[grp]], w=[P.hya_l[grp]])
    k.release(mark)
    if P.dbg and P.stage == 3:
        o2 = P.dout("dbg_L0", [64, 128, 128], BF16)
        k.dma("sync", o2, L_scr[0], r=[L_lab[0]], final=True)


def phase4(P):
    k = P.k
    xb = P.inp["xb"]
    ag = P.din("ag", [1, 512]); hg = P.din("hg", [128, 4]); w_o = P.din("w_o", [D, D])
    ln1g = P.din("ln1g", [1, D]); ln1b = P.din("ln1b", [1, D]); wr = P.din("wr", [D, 16])
    hy_all = P.hy_all; hya_l = P.hya_l
    P.x1_scr = P.dscr("x1_scr", [SO, D], F32); P.x1_l = [k.buf(f"x1_{t}") for t in range(16)]
    P.u2_bounce = [P.dscr(f"u2_bounce{c}", [512, D], BF16) for c in range(4)]; u2b_l = [k.buf(f"u2_bounce{c}") for c in range(4)]
    P.aff_bounce = P.dscr("aff_bounce", [SO, 16], F32); affb_l = k.buf("aff_bounce")
    P.u2_all = [P.dscr(f"u2_all{c}", [4 * 512, D], BF16) for c in range(4)]; P.u2a_l = [k.buf(f"u2_all{c}") for c in range(4)]
    P.aff_all = P.dscr("aff_all", [S, 16], F32); P.affa_l = k.buf("aff_all")
    GR = [[0, 1, 2, 3], [4, 5, 6, 7]]

    P.aff_own = k.sb("aff_own", [128, 16, 16], F32)
    P.affo_l = k.buf("aff_own_l")
    mark = k.mark()
    wob = P.load_bf("wob", [128, 8, D], w_o.rearrange("(k p) n -> p k n", p=128))
    ln1g_bc = P.load("ln1g_bc", [128, D], ln1g.to_broadcast([128, D]))
    ln1b_bc = P.load("ln1b_bc", [128, D], ln1b.to_broadcast([128, D]))
    ag_bc = P.load("ag_bc", [128, 512], ag.to_broadcast([128, 512]))
    hgt = P.load("hgt", [128, 4], hg)
    wrt = P.load("wrt", [128, 8, 16], wr.rearrange("(k p) n -> p k n", p=128))
    hyo = k.sb("hyo", [128, 4, 512], F32)
    hnT = k.sb("hnT", [128, 4, 512], BF16)
    sqh = [k.sb(f"sqh{i}", [128, 512], BF16) for i in range(2)]
    rstdh = k.sb("rstdh", [128, 512], F32)
    junk = k.sb("junk", [128, D], F32)
    st = k.sb("st4", [128, 8], F32)
    an = k.sb("an", [128, 512], BF16)
    anT = k.sb("anT", [128, 4, 128], BF16)
    xo = [k.sb(f"xo{i}", [128, D], F32) for i in range(2)]
    v = k.sb("v4", [128, D], F32)
    x1 = [k.sb(f"x1t{i}", [128, D], F32) for i in range(2)]
    u2 = k.sb("u2t", [128, D], F32)
    u2b = [k.sb(f"u2b{i}", [128, D], BF16) for i in range(2)]
    u2T = k.sb("u2T", [128, 8, 128], F32)
    ex = k.sb("ex", [128, 16], F32)
    for tg in range(4):
        for g4 in range(4):
            P.dyn_dma("sync", hyo.t[g4 * 32:(g4 + 1) * 32, :, :],
                      lambda base, tg=tg, g4=g4: hy_all[g4].rearrange("(k c) t -> c k t", c=32)[:, :, bass.ds(base, SO)][:, :, tg * 512:(tg + 1) * 512],
                      r=[hya_l[g4]], w=[hyo])
        ss = P.nps()
        for kc in range(4):
            sq = sqh[kc % 2]
            P.act(sq.t[:], hyo.t[:, kc, :], AF.Square, r=[hyo], w=[sq])
            P.mm(ss.t[:], P.ones_b.t[:], sq.t[:], kc == 0, kc == 3, r=[P.ones_b, sq], w=[ss])
        P.rsqrt(rstdh.t[:], ss.t[:], 1.0 / 512, r=[ss], w=[rstdh])
        for kc in range(4):
            P.stt(hnT.t[:, kc, :], hyo.t[:, kc, :], hgt.t[:, kc:kc + 1], rstdh.t[:], ALU.mult, ALU.mult, r=[hyo, hgt, rstdh], w=[hnT])
        for t4 in range(4):
            t = tg * 4 + t4
            xt_ = xo[t % 2]
            P.dyn_dma("sync", xt_.t[:], lambda base, t=t: xb[bass.ds(base, SO), :][t * 128:(t + 1) * 128, :], w=[xt_])
            P.act(junk.t[:, 0:512], P.a_tok.t[:, t, :], AF.Square, r=[P.a_l[t]], w=[junk, st], accum_out=st.t[:, 0:1])
            P.rsqrt(st.t[:, 1:2], st.t[:, 0:1], 1.0 / 512, r=[st], w=[st])
            P.stt(an.t[:], P.a_tok.t[:, t, :], st.t[:, 1:2], ag_bc.t[:], ALU.mult, ALU.mult, r=[P.a_l[t], st, ag_bc], w=[an])
            ps = P.nps()
            psb = ps.t[:].bitcast(BF16)
            for kc in range(4):
                P.tr(psb[:, kc * 128:(kc + 1) * 128], an.t[:, kc * 128:(kc + 1) * 128], P.ident_b.t[:], r=[an, P.ident_b], w=[ps])
            P.cp(anT.t[:].rearrange("p k t -> p (k t)"), psb[:, 0:512], r=[ps], w=[anT], eng="scalar")
            for half in range(2):
                hs = slice(half * 512, (half + 1) * 512)
                pm = P.nps()
                for kc in range(4):
                    P.mm(pm.t[:], anT.t[:, kc, :], wob.t[:, kc, hs], kc == 0, False, r=[anT, wob], w=[pm])
                for kc in range(4):
                    P.mm(pm.t[:], hnT.t[:, kc, t4 * 128:(t4 + 1) * 128], wob.t[:, 4 + kc, hs], False, kc == 3, r=[hnT, wob], w=[pm])
                P.tt(junk.t[:, hs], pm.t[:], P.modR.t[:, 0, hs], ALU.mult, r=[pm, P.modR], w=[junk])
                P.stt(v.t[:, hs], xt_.t[:, hs], ALPHA, junk.t[:, hs], ALU.mult, ALU.add, r=[xt_, junk], w=[v])
            x1t = x1[t % 2]
            P.layer_norm(x1t, v, ln1g_bc, ln1b_bc, st, junk)
            k.dma("sync", P.x1_scr[t * 128:(t + 1) * 128, :], x1t.t[:], r=[x1t], w=[P.x1_l[t]])
            P.tt(u2.t[:], x1t.t[:], P.modR.t[:, 2, :], ALU.mult, r=[x1t, P.modR], w=[u2])
            P.tt(u2.t[:], u2.t[:], P.modR.t[:, 1, :], ALU.add, r=[u2, P.modR], w=[u2], eng="gpsimd")
            ub = u2b[t % 2]
            P.cp(ub.t[:], u2.t[:], r=[u2], w=[ub], eng="scalar")
            k.dma("sync", P.u2_bounce[tg][t4 * 128:(t4 + 1) * 128, :], ub.t[:], r=[ub], w=[u2b_l[tg]])
            for hb in range(2):
                ps = P.nps()
                for i in range(4):
                    kc = hb * 4 + i
                    P.tr(ps.t[:, i * 128:(i + 1) * 128], u2.t[:, kc * 128:(kc + 1) * 128], P.ident_f.t[:], r=[u2, P.ident_f], w=[ps])
                P.cp(u2T.t[:, hb * 4:(hb + 1) * 4, :].rearrange("p k t -> p (k t)"), ps.t[:], r=[ps], w=[u2T], eng=("scalar" if hb else "vector"))
            pl = P.nps()
            for kc in range(8):
                P.mm(pl.t[:, 0:16], u2T.t[:, kc, :], wrt.t[:, kc, :], kc == 0, kc == 7, r=[u2T, wrt], w=[pl])
            k.op("vector", lambda e, pl=pl: e.tensor_reduce(out=st.t[:, 4:5], in_=pl.t[:, 0:16], axis=AX.X, op=ALU.max), r=[pl], w=[st])
            P.ts(st.t[:, 5:6], st.t[:, 4:5], -1.0, None, ALU.mult, None, r=[st], w=[st])
            P.act(ex.t[:], pl.t[:, 0:16], AF.Exp, r=[pl, st], w=[ex, st], bias=st.t[:, 5:6], accum_out=st.t[:, 6:7])
            k.op("vector", lambda e: e.reciprocal(out=st.t[:, 7:8], in_=st.t[:, 6:7]), r=[st], w=[st])
            P.ts(P.aff_own.t[:, t, :], ex.t[:], st.t[:, 7:8], None, ALU.mult, None, r=[ex, st], w=[P.affo_l])
            k.dma("sync", P.aff_bounce[t * 128:(t + 1) * 128, :], P.aff_own.t[:, t, :], r=[P.affo_l], w=[affb_l])
            if t4 == 3:
                k.collective("AllGather", ALU.bypass, GR, ins=[P.u2_bounce[tg]], outs=[P.u2_all[tg]], r=[u2b_l[tg]], w=[P.u2a_l[tg]])
    k.collective("AllGather", ALU.bypass, GR, ins=[P.aff_bounce], outs=[P.aff_all], r=[affb_l], w=[P.affa_l])
    k.release(mark)
    if P.dbg and P.stage == 4:
        o = P.dout("dbg_x1", [SO, D]); k.dma("sync", o, P.x1_scr, r=P.x1_l, final=True)
        o = P.dout("dbg_aff", [S, 16]); k.dma("sync", o, P.aff_all, r=[P.affa_l], final=True)
        pass


def _layer_norm(P, out, v, g_bc, b_bc, st, junk):
    k = P.k
    k.op("vector", lambda e: e.tensor_reduce(out=st.t[:, 0:1], in_=v.t[:], axis=AX.X, op=ALU.add), r=[v], w=[st])
    P.ts(st.t[:, 1:2], st.t[:, 0:1], -1.0 / D, None, ALU.mult, None, r=[st], w=[st])
    P.ts(v.t[:], v.t[:], st.t[:, 1:2], None, ALU.add, None, r=[v, st], w=[v])
    P.act(junk.t[:], v.t[:], AF.Square, r=[v], w=[junk, st], accum_out=st.t[:, 2:3])
    P.rsqrt(st.t[:, 3:4], st.t[:, 2:3], 1.0 / D, r=[st], w=[st])
    P.stt(out.t[:], v.t[:], st.t[:, 3:4], g_bc.t[:], ALU.mult, ALU.mult, r=[v, st, g_bc], w=[out])
    P.tt(out.t[:], out.t[:], b_bc.t[:], ALU.add, r=[out, b_bc], w=[out], eng="gpsimd")


Prog.layer_norm = _layer_norm

BIG = float(1 << 20)
CAP = 1024


def phase5(P):
    k = P.k
    GR = [[0, 1, 2, 3], [4, 5, 6, 7]]
    wg = P.din("wg", [4, D, D]); wu = P.din("wu", [4, D, D]); wd = P.din("wd", [4, D, D])
    P.pos_scr = P.dscr("pos_scr", [S, 16], I32); P.pos_l = k.buf("pos_scr")
    xe_dram = [P.dscr(f"xe_dram{i}", [CAP + 128, D], BF16) for i in range(4)]; xe_l = [k.buf(f"xe{i}") for i in range(4)]
    ye_bounce = [[P.dscr(f"ye_b{i}_{h}", [512, D], BF16) for h in range(2)] for i in range(4)]
    ye_allc = [[P.dscr(f"ye_allc{i}_{h}", [4 * 512, D], BF16) for h in range(2)] for i in range(4)]
    P.ye_full = [P.dscr(f"ye_full{i}", [4 * CAP, D], BF16) for i in range(4)]
    yeb_l = [[k.buf(f"yeb{i}{h}") for h in range(2)] for i in range(4)]
    yec_l = [[k.buf(f"yec{i}{h}") for h in range(2)] for i in range(4)]
    P.yea_l = [k.buf(f"yea{i}") for i in range(4)]
    mark = k.mark()
    affT = k.sb("affT", [128, 64, 16], F32)
    k.dma("sync", affT.t[:], P.aff_all.rearrange("(f p) e -> p f e", p=128), r=[P.affa_l], w=[affT])
    A = k.sb("A5", [128, 16, 64], F32)
    P.cp(A.t[:], affT.t[:].rearrange("p f e -> p e f"), r=[affT], w=[A])
    ones_f = k.sb("ones_f", [128, 128], F32); P.memset(ones_f.t[:], 1.0, w=[ones_f])
    U = k.sb("U5", [128, 128], F32)
    P.memset(U.t[:], 1.0, w=[U], eng="gpsimd")
    k.op("gpsimd", lambda e: e.affine_select(out=U.t[:], in_=U.t[:], pattern=[[1, 128]], compare_op=ALU.is_gt, fill=0.0,
                                             base=0, channel_multiplier=-1), r=[U], w=[U])
    Ub = k.sb("Ub", [128, 128], BF16); P.cp(Ub.t[:], U.t[:], r=[U], w=[Ub])
    lo = k.sb("lo", [128, 16], F32); hi = k.sb("hi", [128, 16], F32); mid = k.sb("mid", [128, 16], F32)
    cnt = k.sb("cnt", [128, 16], F32); ge = k.sb("ge", [128, 16], F32); d1 = k.sb("d1", [128, 16], F32)
    cmp = k.sb("cmp", [128, 16, 64], F32)
    P.memset(lo.t[:], 0.0, w=[lo]); P.memset(hi.t[:], 1.0, w=[hi])
    for it in range(30):
        P.tt(mid.t[:], lo.t[:], hi.t[:], ALU.add, r=[lo, hi], w=[mid])
        P.ts(mid.t[:], mid.t[:], 0.5, None, ALU.mult, None, r=[mid], w=[mid])
        P.tt(cmp.t[:], A.t[:], mid.t[:].unsqueeze(2).to_broadcast([128, 16, 64]), ALU.is_ge, r=[A, mid], w=[cmp])
        k.op("vector", lambda e: e.tensor_reduce(out=cnt.t[:], in_=cmp.t[:], axis=AX.X, op=ALU.add), r=[cmp], w=[cnt])
        pt = P.nps()
        P.mm(pt.t[:, 0:16], ones_f.t[:], cnt.t[:], True, True, r=[ones_f, cnt], w=[pt])
        P.ts(ge.t[:], pt.t[:, 0:16], CAP - 0.5, None, ALU.is_ge, None, r=[pt], w=[ge])
        P.tt(d1.t[:], mid.t[:], lo.t[:], ALU.subtract, r=[mid, lo], w=[d1])
        P.tt(d1.t[:], d1.t[:], ge.t[:], ALU.mult, r=[d1, ge], w=[d1])
        P.tt(lo.t[:], lo.t[:], d1.t[:], ALU.add, r=[lo, d1], w=[lo])
        P.tt(d1.t[:], hi.t[:], mid.t[:], ALU.subtract, r=[hi, mid], w=[d1])
        P.tt(d1.t[:], d1.t[:], ge.t[:], ALU.mult, r=[d1, ge], w=[d1])
        P.tt(hi.t[:], mid.t[:], d1.t[:], ALU.add, r=[mid, d1], w=[hi])
    P.thr = lo
    mask = cmp
    P.tt(mask.t[:], A.t[:], lo.t[:].unsqueeze(2).to_broadcast([128, 16, 64]), ALU.is_ge, r=[A, lo], w=[mask])
    maskb = k.sb("maskb", [128, 1024], BF16)
    P.cp(maskb.t[:], mask.t[:].rearrange("p e f -> p (e f)"), r=[mask], w=[maskb])
    pre = k.sb("pre5", [128, 16, 64], F32)
    s0 = k.sb("s0", [128, 16, 64], F32); s1 = k.sb("s1", [128, 16, 64], F32)
    for hh in range(2):
        pp, pc = P.nps(), P.nps()
        P.mm(pp.t[:], Ub.t[:], maskb.t[:, hh * 512:(hh + 1) * 512], True, True, r=[Ub, maskb], w=[pp])
        P.mm(pc.t[:], P.ones_b.t[:], maskb.t[:, hh * 512:(hh + 1) * 512], True, True, r=[P.ones_b, maskb], w=[pc])
        P.cp(pre.t[:, hh * 8:(hh + 1) * 8, :].rearrange("p e f -> p (e f)"), pp.t[:], r=[pp], w=[pre])
        P.cp(s0.t[:, hh * 8:(hh + 1) * 8, :].rearrange("p e f -> p (e f)"), pc.t[:], r=[pc], w=[s0], eng="scalar")
    P.tt(pre.t[:], pre.t[:], s0.t[:], ALU.subtract, r=[pre, s0], w=[pre])
    cur, nxt = s0, s1
    for sh in [1, 2, 4, 8, 16, 32]:
        P.cp(nxt.t[:, :, 0:sh], cur.t[:, :, 0:sh], r=[cur], w=[nxt], eng="gpsimd")
        P.tt(nxt.t[:, :, sh:64], cur.t[:, :, sh:64], cur.t[:, :, 0:64 - sh], ALU.add, r=[cur], w=[nxt])
        cur, nxt = nxt, cur
    P.tt(pre.t[:], pre.t[:], cur.t[:], ALU.add, r=[pre, cur], w=[pre])
    P.ts(pre.t[:], pre.t[:], -float(CAP), None, ALU.add, None, r=[pre], w=[pre])
    P.tt(pre.t[:], pre.t[:], mask.t[:], ALU.mult, r=[pre, mask], w=[pre])
    P.ts(pre.t[:], pre.t[:], float(CAP), float(CAP), ALU.add, ALU.min, r=[pre], w=[pre])
    posI = k.sb("posI", [128, 64, 16], I32)
    P.cp(posI.t[:].rearrange("p f e -> p e f"), pre.t[:], r=[pre], w=[posI])
    k.dma("sync", P.pos_scr.rearrange("(f p) e -> p f e", p=128), posI.t[:], r=[posI], w=[P.pos_l])
    myp = k.sb("myp", [128, 64, 4], I32)
    P.dyn_dma("scalar", myp.t[:], lambda base: P.pos_scr[:, bass.ds(base, 4)].rearrange("(f p) e -> p f e", p=128), r=[P.pos_l], w=[myp], mult=4)
    u2t = [k.sb(f"u2g{i}", [128, D], BF16) for i in range(3)]
    for f in range(64):
        ut = u2t[f % 3]
        r_, q_, tt2 = f // 16, (f % 16) // 4, f % 4
        k.dma("sync", ut.t[:], P.u2_all[q_][r_ * 512 + tt2 * 128:r_ * 512 + (tt2 + 1) * 128, :], r=[P.u2a_l[q_]], w=[ut])
        for i in range(4):
            k.custom_dma("gpsimd", lambda e, ut=ut, f=f, i=i: e.indirect_dma_start(
                out=xe_dram[i][:, :], out_offset=bass.IndirectOffsetOnAxis(ap=myp.t[:, f, i:i + 1], axis=0),
                in_=ut.t[:, :], in_offset=None), r=[ut, myp], w=[xe_l[i]])
    k.release(mark)
    if P.dbg and P.stage == 5:
        P.dump("thr", lo, lo.t[0:1, :], [1, 16])
    mark = k.mark()
    xe = k.sb("xe", [128, 8, D], BF16)
    xeT = k.sb("xeT", [128, 8, CAP], BF16)
    hT = k.sb("hT", [128, 8, CAP], BF16)
    sil = [k.sb(f"sil{i}", [128, 512], F32) for i in range(2)]
    yeb = [k.sb(f"yeb{i}", [128, D], BF16) for i in range(2)]
    WB = [[k.sb(f"w{n}b{q}", [128, 8, D], BF16) for n in "gud"] for q in range(2)]
    wst = [k.sb(f"wst{i}", [128, D], F32) for i in range(3)]
    wi = 0

    def load_w(i):
        nonlocal wi
        for (wsrc, wdst) in zip((wg, wu, wd), WB[i % 2]):
            wv_ = wsrc[i].rearrange("(k p) n -> p k n", p=128)
            for kc in range(8):
                st = wst[wi % 3]
                k.dma("sync", st.t[:], wv_[:, kc, :], w=[st])
                P.cp(wdst.t[:, kc, :], st.t[:], r=[st], w=[wdst], eng=["vector", "gpsimd", "scalar"][wi % 3])
                wi += 1

    load_w(0)
    for i in range(4):
        wgb, wub, wdb = WB[i % 2]
        k.dma("sync", xe.t[:], xe_dram[i][0:CAP, :].rearrange("(s p) d -> p s d", p=128), r=[xe_l[i]], w=[xe])
        if i + 1 < 4:
            load_w(i + 1)
        for kc in range(8):
            ps = P.nps()
            psb = ps.t[:].bitcast(BF16)
            for st_ in range(8):
                P.tr(psb[:, st_ * 128:(st_ + 1) * 128], xe.t[:, st_, kc * 128:(kc + 1) * 128], P.ident_b.t[:], r=[xe, P.ident_b], w=[ps])
            P.cp(xeT.t[:, kc, :], psb[:, :], r=[ps], w=[xeT], eng=("scalar" if kc % 2 else "vector"))
        for fc in range(8):
            for hf in range(2):
                hs = slice(hf * 512, (hf + 1) * 512)
                pg, pu = P.nps(), P.nps()
                for kc in range(8):
                    P.mm(pg.t[:], wgb.t[:, kc, fc * 128:(fc + 1) * 128], xeT.t[:, kc, hs], kc == 0, kc == 7, r=[wgb, xeT], w=[pg])
                for kc in range(8):
                    P.mm(pu.t[:], wub.t[:, kc, fc * 128:(fc + 1) * 128], xeT.t[:, kc, hs], kc == 0, kc == 7, r=[wub, xeT], w=[pu])
                sl_ = sil[(fc * 2 + hf) % 2]
                P.act(sl_.t[:], pg.t[:], AF.Silu, r=[pg], w=[sl_])
                P.tt(hT.t[:, fc, hs], sl_.t[:], pu.t[:], ALU.mult, r=[sl_, pu], w=[hT])
        for st_ in range(8):
            yb = yeb[st_ % 2]
            for hh in range(2):
                py = P.nps()
                for fc in range(8):
                    P.mm(py.t[:], hT.t[:, fc, st_ * 128:(st_ + 1) * 128], wdb.t[:, fc, hh * 512:(hh + 1) * 512], fc == 0, fc == 7, r=[hT, wdb], w=[py])
                P.cp(yb.t[:, hh * 512:(hh + 1) * 512], py.t[:], r=[py], w=[yb], eng=("scalar" if hh else "vector"))
            sh = st_ // 4
            k.dma("sync", ye_bounce[i][sh][(st_ % 4) * 128:(st_ % 4 + 1) * 128, :], yb.t[:], r=[yb], w=[yeb_l[i][sh]])
        for sh in range(2):
            k.collective("AllGather", ALU.bypass, GR, ins=[ye_bounce[i][sh]], outs=[ye_allc[i][sh]], r=[yeb_l[i][sh]], w=[yec_l[i][sh]])
    for i in range(4):
        for sh in range(2):
            k.dma("sync", P.ye_full[i].rearrange("(r s q) d -> r s q d", r=4, s=2)[:, sh],
                  ye_allc[i][sh].rearrange("(r q) d -> r q d", r=4), r=[yec_l[i][sh]], w=[P.yea_l[i]])
    k.release(mark)


def phase6(P):
    k = P.k
    ln2g = P.din("ln2g", [1, D]); ln2b = P.din("ln2b", [1, D])
    y = P.dout("y", [SO, D])
    mark = k.mark()
    ln2g_bc = P.load("ln2g_bc", [128, D], ln2g.to_broadcast([128, D]))
    ln2b_bc = P.load("ln2b_bc", [128, D], ln2b.to_broadcast([128, D]))
    posO = k.sb("posO", [128, 16, 16], I32)
    P.dyn_dma("sync", posO.t[:], lambda base: P.pos_scr[bass.ds(base, SO), :].rearrange("(t p) e -> p t e", p=128), r=[P.pos_l], w=[posO])
    posF = k.sb("posF", [128, 16, 16], F32)
    P.cp(posF.t[:], posO.t[:], r=[posO], w=[posF])
    gw = k.sb("gw", [128, 16, 16], F32)
    P.ts(gw.t[:], posF.t[:], CAP - 0.5, None, ALU.is_lt, None, r=[posF], w=[gw])
    P.tt(posF.t[:], posF.t[:], gw.t[:], ALU.mult, r=[posF, gw], w=[posF])
    P.tt(gw.t[:], gw.t[:], P.aff_own.t[:], ALU.mult, r=[gw, P.affo_l], w=[gw])
    for r_ in range(1, 4):
        P.ts(posF.t[:, :, 4 * r_:4 * r_ + 4], posF.t[:, :, 4 * r_:4 * r_ + 4], float(r_ * CAP), None, ALU.add, None, r=[posF], w=[posF])
    rowI = k.sb("rowI", [128, 16, 16], I32)
    P.cp(rowI.t[:], posF.t[:], r=[posF], w=[rowI])
    NB = 8
    G = [k.sb(f"G{i}", [128, D], BF16) for i in range(NB)]
    for g_ in G:
        P.memset(g_.t[:], 0.0, w=[g_])
    acc = k.sb("acc6", [128, D], F32)
    x1t = [k.sb(f"x1r{i}", [128, D], F32) for i in range(2)]
    v = k.sb("v6", [128, D], F32)
    junk = k.sb("junk6", [128, D], F32)
    st = k.sb("st6", [128, 8], F32)
    outt = [k.sb(f"out{i}", [128, D], F32) for i in range(2)]
    gi = 0
    for t in range(16):
        xt_ = x1t[t % 2]
        k.dma("sync", xt_.t[:], P.x1_scr[t * 128:(t + 1) * 128, :], r=[P.x1_l[t]], w=[xt_])
        P.memset(acc.t[:], 0.0, w=[acc], eng="gpsimd")
        for e_ in range(16):
            g_ = G[gi % NB]
            gi += 1
            i_loc = e_ % 4
            k.custom_dma("gpsimd", lambda e, g_=g_, t=t, e_=e_, i_loc=i_loc: e.indirect_dma_start(
                out=g_.t[:, :], out_offset=None,
                in_=P.ye_full[i_loc][:, :], in_offset=bass.IndirectOffsetOnAxis(ap=rowI.t[:, t, e_:e_ + 1], axis=0),
                ), r=[P.yea_l[i_loc], rowI, g_], w=[g_])
            P.stt(acc.t[:], g_.t[:], gw.t[:, t, e_:e_ + 1], acc.t[:], ALU.mult, ALU.add, r=[g_, gw, acc], w=[acc])
        P.tt(junk.t[:], acc.t[:], P.modR.t[:, 3, :], ALU.mult, r=[acc, P.modR], w=[junk], eng="gpsimd")
        P.stt(v.t[:], xt_.t[:], ALPHA, junk.t[:], ALU.mult, ALU.add, r=[xt_, junk], w=[v])
        ot = outt[t % 2]
        P.layer_norm(ot, v, ln2g_bc, ln2b_bc, st, junk)
        k.dma("sync", y[t * 128:(t + 1) * 128, :], ot.t[:], r=[ot], final=True)
    k.release(mark)


_CACHE = {}


def kernel(**inputs):
    inp = {k_: np.asarray(v) for k_, v in inputs.items()}
    if "prog" not in _CACHE:
        P = Prog(stage=9, dbg=False)
        P.hc = host_consts()
        build_all(P)
        P.k.emit()
        _CACHE["prog"] = P
    P = _CACHE["prog"]
    in_maps = []
    for c in range(8):
        hp = host_prep(inp, c)
        hp.update(P.hc)
        in_maps.append({n: np.ascontiguousarray(hp[n]) for n in P.inp})
    res = run_bass_kernel_spmd(P.nc, in_maps, core_ids=list(range(8)))
    out = np.zeros((2, S, D), np.float32)
    for c in range(8):
        b, j = c // 4, c % 4
        out[b, j * SO:(j + 1) * SO, :] = np.asarray(res.results[c]["y"], dtype=np.float32)
    return out
```

```python
from contextlib import ExitStack
import concourse.bass as bass
import concourse.mybir as mybir

_DT_SIZE = {"float32": 4, "bfloat16": 2, "int32": 4, "uint32": 4, "float16": 2, "uint8": 1, "int8": 1,
            "uint16": 2, "int16": 2}


def _dsize(dt):
    n = getattr(dt, "name", None) or str(dt)
    for k_, v in _DT_SIZE.items():
        if k_ in str(n):
            return v
    raise ValueError(f"dtype {dt}")


class Buf:
    __slots__ = ("name", "t", "last_w", "readers")

    def __init__(self, name, t=None):
        self.name = name
        self.t = t
        self.last_w = None
        self.readers = []


class Op:
    __slots__ = ("eng", "fn", "deps", "kind", "awaited", "sem", "semval", "idx", "final", "dclass")

    def __init__(self, eng, fn, kind):
        self.eng = eng
        self.fn = fn
        self.deps = []
        self.kind = kind
        self.awaited = False
        self.sem = None
        self.semval = None
        self.final = False
        self.dclass = None


ENGS = ["sync", "scalar", "vector", "gpsimd", "tensor"]
NDMA = 8
import os
CC_INC = int(os.environ.get("CC_INC", "1"))


class K:
    def __init__(self, nc, sbuf_base=16640, sbuf_limit=None):
        self.nc = nc
        self.ops = {e: [] for e in ENGS}
        self.stack = ExitStack()
        self.sb_off = sbuf_base
        self.sb_limit = sbuf_limit if sbuf_limit is not None else 16512 + nc.sbuf_bytes_remaining
        self.sb_hw = sbuf_base
        self.sb_epoch = 0
        self.dma_count = {e: 0 for e in ENGS}
        self.dma_last = {}
        self.barrier_ops = {e: [] for e in ENGS}
        self.finals = []
        self.n_names = 0
        self.ps_live = []

    def sb(self, name, shape, dtype):
        nbytes = _dsize(dtype)
        for s in shape[1:]:
            nbytes *= s
        off = (self.sb_off + 63) // 64 * 64
        assert off + nbytes <= self.sb_limit, f"SBUF overflow at {name}: {off}+{nbytes}"
        self.n_names += 1
        t = self.nc.alloc_sbuf_tensor_at(f"{name}_{self.n_names}", list(shape), dtype, offset=off)
        self.sb_off = off + nbytes
        self.sb_hw = max(self.sb_hw, self.sb_off)
        return Buf(name, t)

    def mark(self):
        return self.sb_off

    def release(self, mark):
        self.sb_off = mark
        self.sb_epoch += 1
        self.barrier()

    def ps(self, name, shape, dtype):
        self.n_names += 1
        t = self.stack.enter_context(self.nc.psum_tensor(f"{name}_{self.n_names}", list(shape), dtype))
        return Buf(name, t)

    def buf(self, name):
        return Buf(name)

    def _add(self, op, r, w):
        eng = op.eng
        deps = []
        for b in r:
            if b.last_w is not None:
                deps.append(b.last_w)
        for b in w:
            if b.last_w is not None:
                deps.append(b.last_w)
            for rd in b.readers:
                deps.append(rd)
        if self.barrier_ops[eng]:
            deps.extend(self.barrier_ops[eng])
            self.barrier_ops[eng] = []
        seen = set()
        for d in deps:
            if d is op or id(d) in seen:
                continue
            seen.add(id(d))
            if d.eng == eng and d.kind == "compute":
                if eng == "tensor":
                    continue
            op.deps.append(d)
        for b in r:
            b.readers.append(op)
        for b in w:
            b.last_w = op
            b.readers = []
        self.ops[eng].append(op)
        return op

    def op(self, eng, fn, r=(), w=()):
        return self._add(Op(eng, fn, "compute"), list(r), list(w))

    def dma(self, eng, out, in_, r=(), w=(), final=False, **kw):
        op = Op(eng, lambda e: e.dma_start(out=out, in_=in_, **kw), "dma")
        cls = self.dma_count[eng] % NDMA
        self.dma_count[eng] += 1
        op.dclass = cls
        prev = self.dma_last.get((eng, cls))
        self._add(op, list(r), list(w))
        if prev is not None and prev not in op.deps:
            op.deps.append(prev)
        self.dma_last[(eng, cls)] = op
        if final:
            op.final = True
            self.finals.append(op)
        return op

    def custom_dma(self, eng, fn, r=(), w=(), final=False):
        op = Op(eng, fn, "dma")
        cls = self.dma_count[eng] % NDMA
        self.dma_count[eng] += 1
        op.dclass = cls
        prev = self.dma_last.get((eng, cls))
        self._add(op, list(r), list(w))
        if prev is not None and prev not in op.deps:
            op.deps.append(prev)
        self.dma_last[(eng, cls)] = op
        if final:
            op.final = True
            self.finals.append(op)
        return op

    def collective(self, kind, alu, groups, ins, outs, r=(), w=()):
        op = Op("gpsimd", lambda e: e.collective_compute(kind, alu, replica_groups=groups, ins=ins, outs=outs), "cc")
        self._add(op, list(r), list(w))
        return op

    def barrier(self):
        lasts = []
        for e in ENGS:
            if self.ops[e]:
                lasts.append(self.ops[e][-1])
        for key, op in self.dma_last.items():
            lasts.append(op)
        for e in ENGS:
            self.barrier_ops[e] = list(lasts)

    def emit(self):
        nc = self.nc
        tail_deps = list(self.finals)
        for d in tail_deps:
            d.awaited = True
        for e in ENGS:
            for op in self.ops[e]:
                for d in op.deps:
                    d.awaited = True
        with ExitStack() as st:
            esem = {e: st.enter_context(nc.semaphore(f"s_{e}")) for e in ENGS}
            dsem = {(e, c): st.enter_context(nc.semaphore(f"d_{e}_{c}")) for e in ENGS if self.dma_count[e] > 0
                    for c in range(min(NDMA, self.dma_count[e]))}
            for e in ENGS:
                cnt = 0
                dcnt = {}
                for op in self.ops[e]:
                    if op.kind == "compute":
                        if op.awaited:
                            cnt += 1
                            op.sem = esem[e]
                            op.semval = cnt
                    elif op.kind == "cc":
                        op.sem = st.enter_context(nc.semaphore(f"cc_{id(op)}"))
                        op.semval = CC_INC
                        op.awaited = True
                    else:
                        c = op.dclass
                        dcnt[c] = dcnt.get(c, 0) + 16
                        op.sem = dsem[(e, c)]
                        op.semval = dcnt[c]
                        op.awaited = True
            block = st.enter_context(nc.Block())
            handles = {"sync": block.sync, "scalar": block.scalar, "vector": block.vector,
                       "gpsimd": block.gpsimd, "tensor": block.tensor}

            def make(e):
                def body(eng):
                    seen = {}
                    for op in self.ops[e]:
                        for d in op.deps:
                            key = id(d.sem)
                            if seen.get(key, 0) >= d.semval:
                                continue
                            eng.wait_ge(d.sem, d.semval)
                            seen[key] = d.semval
                        ins = op.fn(eng)
                        if op.kind == "compute":
                            if op.awaited:
                                ins.then_inc(op.sem, 1)
                        elif op.kind == "cc":
                            ins.then_inc(op.sem, CC_INC)
                        else:
                            ins.then_inc(op.sem, 16)
                    if e == "sync":
                        for d in tail_deps:
                            key = id(d.sem)
                            if seen.get(key, 0) >= d.semval:
                                continue
                            eng.wait_ge(d.sem, d.semval)
                            seen[key] = d.semval
                return body

            for e in ENGS:
                if self.ops[e] or e == "sync":
                    handles[e](make(e))
        self.stack.close()


import os
import numpy as np
import ml_dtypes
import concourse.bass as bass
import concourse.mybir as mybir
from concourse.bass_utils import run_bass_kernel_spmd

F32 = mybir.dt.float32
BF16 = mybir.dt.bfloat16
I32 = mybir.dt.int32
AF = mybir.ActivationFunctionType
ALU = mybir.AluOpType
AX = mybir.AxisListType

D = 1024
S = 8192
SO = 2048
H = 8
EPS = 1e-5
ALPHA = 2.0 ** 0.25
PI = float(np.pi)
MAGIC = 12582912.0
SCALE = 96.0 ** -0.5
NFFT = 16384


def host_consts():
    c = {}
    n = np.arange(128, dtype=np.float64)
    ang128 = 2 * np.pi * np.outer(n, n) / 128.0
    C2 = np.cos(ang128)
    S2 = np.sin(ang128)
    c["F1"] = np.concatenate([C2, -S2], 1).astype(np.float32)
    c["C2"] = C2.astype(np.float32)
    c["S2"] = S2.astype(np.float32)
    c["S2n"] = (-S2).astype(np.float32)
    c["G1"] = np.concatenate([C2, S2], 1).astype(np.float32)
    c["G2"] = np.concatenate([-S2, C2], 1).astype(np.float32)
    th = 2 * np.pi * np.outer(n, n) / NFFT
    c["TC"] = np.cos(th).astype(np.float32)
    c["TS"] = np.sin(th).astype(np.float32)
    c["TSn"] = (-np.sin(th)).astype(np.float32)
    c["CI"] = (C2[:, :64] / NFFT).astype(np.float32)
    c["SIn"] = (-S2[:, :64] / NFFT).astype(np.float32)
    L = S
    m = np.arange(NFFT)
    lag = np.where(m < L, m, NFFT - m)
    lag = np.where(m == L, 0, lag)
    pos = lag.astype(np.float32)
    t = pos / np.float32(L - 1)
    bands = 16
    freqs = np.linspace(1e-4, bands - 1, bands, dtype=np.float32)
    phase = (np.float32(2.0 * np.pi / L) * pos[:, None]) * freqs[None, :]
    feats = np.concatenate([t[:, None], np.cos(phase), -np.sin(phase)], -1).astype(np.float32)
    c["featsT"] = np.ascontiguousarray(feats.T)
    n1 = np.arange(128)
    e1 = np.where(n1 < 64, 128.0 * n1, NFFT - 128.0 * n1) / (L - 1)
    c["e1s"] = (-e1).astype(np.float32).reshape(128, 1)
    c["n2row"] = np.broadcast_to(np.arange(128, dtype=np.float32)[None, :], (128, 128)).copy()
    inv_freq = 10000.0 ** (-np.arange(16, dtype=np.float32) / 16.0)
    invf = np.zeros((128, 1), np.float32)
    sgn = np.zeros((128, 1), np.float32)
    invf[64:80, 0] = inv_freq
    invf[80:96, 0] = inv_freq
    sgn[64:80, 0] = -1.0
    sgn[80:96, 0] = 1.0
    c["invf"] = invf
    c["sgn"] = sgn
    return c


def fm(v, k):
    return np.ascontiguousarray(np.asarray(v).reshape(k, 128).T)


def host_prep(inp, core):
    b, j = core // 4, core % 4
    l = 0
    o = {}
    o["xb"] = np.ascontiguousarray(inp["x"][b])
    o["cvec"] = fm(inp["c"][b], 8)
    o["posf"] = np.ascontiguousarray(inp["positions"][b].reshape(1, S).astype(np.int32))
    o["w_ada"] = np.ascontiguousarray(inp["w_ada"][l])
    o["b_ada"] = np.ascontiguousarray(inp["b_ada"][l].reshape(1, 6 * D))
    w_in = inp["w_in"][l]
    W1 = np.zeros((D, 768), np.float32)
    W1[:, 0:128] = w_in[:, 256:384]
    W1[:, 128 + 64:128 + 96] = w_in[:, 384:416]
    W1[:, 256 + 64:256 + 80] = w_in[:, 400:416]
    W1[:, 256 + 80:256 + 96] = w_in[:, 384:400]
    for g in range(3):
        c0 = 416 + g * 512 + 128 * j
        W1[:, 384 + g * 128:384 + (g + 1) * 128] = w_in[:, c0:c0 + 128]
    o["W1"] = W1
    o["wcq"] = np.ascontiguousarray(w_in[:, 0:256])
    o["qg"] = fm(inp["q_norm_g"][l], 2)
    wqb = inp["w_qb"][l]
    wq = np.zeros((256, H, 2, 128), np.float32)
    for h in range(H):
        wq[:, h, 0, 0:64] = wqb[:, h * 96:h * 96 + 64]
        wq[:, h, 0, 64:96] = wqb[:, h * 96 + 64:h * 96 + 96]
        wq[:, h, 1, 64:80] = wqb[:, h * 96 + 80:h * 96 + 96]
        wq[:, h, 1, 80:96] = wqb[:, h * 96 + 64:h * 96 + 80]
    o["wq"] = wq.reshape(256, H * 2 * 128)
    o["kvg"] = fm(inp["kv_norm_g"][l], 1)
    wkvb = inp["w_kvb"][l]
    wk = np.zeros((128, H, 128), np.float32)
    wv = np.zeros((128, H, 64), np.float32)
    for h in range(H):
        wk[:, h, 0:64] = wkvb[:, h * 128:h * 128 + 64]
        wv[:, h, :] = wkvb[:, h * 128 + 64:h * 128 + 128]
    o["wk"] = wk.reshape(128, H * 128)
    o["wv"] = wv.reshape(128, H * 64)
    cw = inp["conv_w"][l]
    cb = inp["conv_b"][l]
    cwl = np.zeros((128, 3, 3), np.float32)
    cbl = np.zeros((128, 3), np.float32)
    for g in range(3):
        sl = slice(g * 512 + 128 * j, g * 512 + 128 * j + 128)
        cwl[:, g, :] = cw[:, sl].T
        cbl[:, g] = cb[sl]
    o["cw"] = cwl.reshape(128, 9)
    o["cb"] = cbl
    o["fw1"] = np.ascontiguousarray(inp["filt_w1"][l])
    o["fb1"] = np.ascontiguousarray(inp["filt_b1"][l].reshape(64, 1))
    o["ffreq"] = np.ascontiguousarray(inp["filt_freq"][l].reshape(64, 1))
    o["fw2"] = np.ascontiguousarray(inp["filt_w2"][l])
    o["fb2"] = np.ascontiguousarray(inp["filt_b2"][l].reshape(64, 1))
    w3 = inp["filt_w3"][l].reshape(64, 2, 2, 512)
    b3 = inp["filt_b3"][l].reshape(2, 2, 512)
    dec = inp["hyena_decay"][l]
    cs = slice(128 * j, 128 * j + 128)
    w3l = w3[:, :, :, cs].reshape(64, 2, 2, 4, 32).transpose(0, 2, 3, 1, 4)
    o["fw3"] = np.ascontiguousarray(w3l.reshape(64, 512))
    b3l = b3[:, :, cs].reshape(2, 2, 4, 32).transpose(1, 2, 0, 3)
    o["fb3"] = np.ascontiguousarray(b3l.reshape(1, 512))
    dl = dec[:, :, cs].reshape(2, 2, 4, 32).transpose(1, 2, 0, 3)
    o["fdec"] = np.ascontiguousarray(dl.reshape(1, 512))
    o["hbias"] = np.ascontiguousarray(inp["hyena_bias"][l][:, cs].reshape(1, 256))
    o["ag"] = np.ascontiguousarray(inp["attn_out_g"][l].reshape(1, 512))
    o["hg"] = fm(inp["hyena_out_g"][l], 4)
    o["w_o"] = np.ascontiguousarray(inp["w_o"][l])
    o["ln1g"] = np.ascontiguousarray(inp["ln1_g"][l].reshape(1, D))
    o["ln1b"] = np.ascontiguousarray(inp["ln1_b"][l].reshape(1, D))
    o["wr"] = np.ascontiguousarray(inp["w_router"][l])
    o["wg"] = np.ascontiguousarray(inp["w_gate"][l][4 * j:4 * j + 4])
    o["wu"] = np.ascontiguousarray(inp["w_up"][l][4 * j:4 * j + 4])
    o["wd"] = np.ascontiguousarray(inp["w_down"][l][4 * j:4 * j + 4])
    o["ln2g"] = np.ascontiguousarray(inp["ln2_g"][l].reshape(1, D))
    o["ln2b"] = np.ascontiguousarray(inp["ln2_b"][l].reshape(1, D))
    return o


INPUT_SHAPES = None


class Prog:
    def __init__(self, stage=99, dbg=False):
        self.stage = stage
        self.dbg = dbg
        self.nc = nc = bass.Bass("TRN2", target_bir_lowering=False)
        self.k = K(nc)
        self.inp = {}
        self.dbg_outs = {}
        self.pid = None
        self._psi = 0
        self._pidc = {}

    def din(self, name, shape, dt=F32):
        self.inp[name] = self.nc.dram_tensor(name, list(shape), dt, kind="ExternalInput").ap()
        return self.inp[name]

    def dout(self, name, shape, dt=F32):
        return self.nc.dram_tensor(name, list(shape), dt, kind="ExternalOutput").ap()

    def dscr(self, name, shape, dt=F32):
        return self.nc.dram_tensor(name, list(shape), dt).ap()

    def dump(self, name, bufs, ap, shape, dt=F32):
        if not self.dbg:
            return
        if not isinstance(bufs, (list, tuple)):
            bufs = [bufs]
        o = self.dout("dbg_" + name, shape, dt)
        self.k.dma("sync", o, ap, r=list(bufs), final=True)

    def mm(self, out, lhsT, rhs, start, stop, r, w):
        self.k.op("tensor", lambda e: e.matmul(out, lhsT=lhsT, rhs=rhs, start=start, stop=stop), r=r, w=w)

    def tr(self, out, in_, ident, r, w):
        self.k.op("tensor", lambda e: e.transpose(out=out, in_=in_, identity=ident), r=r, w=w)

    def act(self, out, in_, func, r, w, eng="scalar", **kw):
        self.k.op("scalar", lambda e: e.activation(out=out, in_=in_, func=func, **kw), r=r, w=w)

    def tt(self, out, in0, in1, op, r, w, eng="vector"):
        self.k.op(eng, lambda e: e.tensor_tensor(out=out, in0=in0, in1=in1, op=op), r=r, w=w)

    def ts(self, out, in0, s1, s2, op0, op1, r, w, eng="vector", **kw):
        if op1 is None:
            self.k.op(eng, lambda e: e.tensor_scalar(out=out, in0=in0, scalar1=s1, scalar2=None, op0=op0, **kw), r=r, w=w)
        else:
            self.k.op(eng, lambda e: e.tensor_scalar(out=out, in0=in0, scalar1=s1, scalar2=s2, op0=op0, op1=op1, **kw), r=r, w=w)

    def stt(self, out, in0, scalar, in1, op0, op1, r, w, eng="vector"):
        self.k.op(eng, lambda e: e.scalar_tensor_tensor(out=out, in0=in0, scalar=scalar, in1=in1, op0=op0, op1=op1), r=r, w=w)

    def rsqrt(self, out, in_, scale, r, w):
        self.act(out, in_, AF.Sqrt, r=list(r) + [self.eps_t], w=w, scale=scale, bias=self.eps_t.t[:, 0:1])
        self.k.op("vector", lambda e: e.reciprocal(out=out, in_=out), r=w, w=w)

    def cp(self, out, in_, r, w, eng="vector"):
        if eng == "scalar":
            self.k.op("scalar", lambda e: e.activation(out=out, in_=in_, func=AF.Copy), r=r, w=w)
        else:
            self.k.op(eng, lambda e: e.tensor_copy(out=out, in_=in_), r=r, w=w)

    def memset(self, ap, val, w, eng="vector"):
        self.k.op(eng, lambda e: e.memset(ap, val), w=w)

    def nps(self):
        b = self.PS[self._psi % len(self.PS)]
        self._psi += 1
        return b

    def load(self, name, shape, src_ap, dt=F32, eng="sync"):
        b = self.k.sb(name, shape, dt)
        self.k.dma(eng, b.t[:], src_ap, w=[b])
        return b

    def load_bf(self, name, shape, src_ap, conv_eng="vector"):
        b = self.k.sb(name, shape, BF16)
        a, n = shape[1], shape[2]
        if getattr(self, "_stg_mark", None) != self.k.sb_epoch or self._stg_n < n:
            self._stg_mark = self.k.sb_epoch
            self._stg = [self.k.sb(f"stg{i}", [128, max(n, 1024)], F32) for i in range(2)]
            self._stg_n = max(n, 1024)
            self._stg_i = 0
        for i in range(a):
            st = self._stg[self._stg_i % 2]
            self._stg_i += 1
            self.k.dma("sync", st.t[:, 0:n], src_ap[:, i, :], w=[st])
            self.cp(b.t[:, i, :], st.t[:, 0:n], r=[st], w=[b], eng=conv_eng)
        return b

    def dyn_dma(self, eng, out, mk_in, r=(), w=(), mult=SO):
        def fn(e):
            key = (eng, mult)
            if key not in self._pidc:
                if (eng, "pid") not in self._pidc:
                    self._pidc[(eng, "pid")] = e.partition_id()
                pid = self._pidc[(eng, "pid")]
                self._pidc[key] = e.snap((pid & 3) * mult, min_val=0, max_val=3 * mult)
            base = self._pidc[key]
            return e.dma_start(out=out, in_=mk_in(base))
        return self.k.custom_dma(eng, fn, r=r, w=w)

    def range_reduce(self, out, ang, tmp, r, w, eng="vector"):
        self.ts(tmp, ang, 1.0 / (2 * PI), MAGIC, ALU.mult, ALU.add, r=r, w=w, eng=eng)
        self.ts(tmp, tmp, -MAGIC, None, ALU.add, None, r=w, w=w, eng=eng)
        if eng == "vector":
            self.stt(out, tmp, -2 * PI, ang, ALU.mult, ALU.add, r=list(r) + list(w), w=w)
        else:
            self.ts(tmp, tmp, -2 * PI, None, ALU.mult, None, r=w, w=w, eng=eng)
            self.tt(out, tmp, ang, ALU.add, r=list(r) + list(w), w=w, eng=eng)
        self.ts(out, out, PI, -PI, ALU.min, ALU.max, r=w, w=w, eng=eng)


def phase01(P):
    k, nc = P.k, P.nc
    hc = P.hc
    xb = P.din("xb", [S, D])
    cvec = P.din("cvec", [128, 8])
    posf = P.din("posf", [1, S], I32)
    w_ada = P.din("w_ada", [D, 6 * D])
    b_ada = P.din("b_ada", [1, 6 * D])
    W1 = P.din("W1", [D, 768])
    wcq = P.din("wcq", [D, 256])
    qg = P.din("qg", [128, 2])
    kvg = P.din("kvg", [128, 1])
    invf = P.din("invf", [128, 1])
    sgn = P.din("sgn", [128, 1])
    P.PS = [k.ps(f"ps{i}", [128, 512], F32) for i in range(8)]
    P.ident_f = k.sb("ident_f", [128, 128], F32)
    P.memset(P.ident_f.t[:], 0.0, w=[P.ident_f], eng="gpsimd")
    k.op("gpsimd", lambda e: e.affine_select(out=P.ident_f.t[:], in_=P.ident_f.t[:], pattern=[[-1, 128]],
                                             compare_op=ALU.not_equal, fill=1.0, base=0, channel_multiplier=1),
         r=[P.ident_f], w=[P.ident_f])
    P.ident_b = k.sb("ident_b", [128, 128], BF16)
    P.cp(P.ident_b.t[:], P.ident_f.t[:], r=[P.ident_f], w=[P.ident_b])
    P.eps_t = k.sb("eps_t", [128, 1], F32)
    P.memset(P.eps_t.t[:], EPS, w=[P.eps_t])
    P.ones_b = k.sb("ones_b", [128, 128], BF16)
    P.memset(P.ones_b.t[:], 1.0, w=[P.ones_b])
    P.invf = P.load("invf", [128, 1], invf)
    P.sgn = P.load("sgn", [128, 1], sgn)
    P.qg = P.load("qg", [128, 2], qg)
    P.kvg = P.load("kvg", [128, 1], kvg)
    P.modR = k.sb("modR", [128, 4, D], F32)
    P.modT = k.sb("modT", [128, 2, 8], F32)
    P.a_tok = k.sb("a_tok", [128, 16, 512], BF16)
    P.a_l = [k.buf(f"a{t}") for t in range(16)]
    P.mark_attn = k.mark()
    P.ckvn = k.sb("ckvn", [128, S], BF16)
    P.krot = k.sb("krot", [128, S], BF16)
    P.hy_scr = P.dscr("hy_scr", [128, 3, S], BF16)
    P.cqn = k.sb("cqn", [128, 2, SO], BF16)
    P.cosO = k.sb("cosO", [128, SO], F32)
    P.sinO = k.sb("sinO", [128, SO], F32)
    ckvn_l = [k.buf(f"ckvn{t}") for t in range(16)]
    krot_l = [k.buf(f"krot{t}") for t in range(16)]
    hy_l = [k.buf(f"hy{t}") for t in range(16)]
    P.hyst = None
    cqn_l = [k.buf(f"cqn{t}") for t in range(4)]
    cs_l = [k.buf(f"cs{t}") for t in range(4)]
    P.ckvn_l, P.krot_l, P.hy_l, P.cqn_l, P.cs_l = ckvn_l, krot_l, hy_l, cqn_l, cs_l
    mark = k.mark()
    cT = P.load("cT", [128, 8], cvec)
    sc = k.sb("sc", [128, 8], F32)
    P.act(sc.t[:], cT.t[:], AF.Silu, r=[cT], w=[sc])
    sc_rep = k.sb("sc_rep", [128, 8, 128], F32)
    P.cp(sc_rep.t[:], sc.t[:].unsqueeze(2).to_broadcast([128, 8, 128]), r=[sc], w=[sc_rep])
    bada = P.load("bada", [128, 6 * D], b_ada.to_broadcast([128, 6 * D]))
    modA = k.sb("modA", [128, 2 * D], F32)
    wa = [k.sb(f"wa{i}", [128, 8, 512], F32) for i in range(2)]
    w_ada_v = w_ada.rearrange("(k p) n -> p k n", p=128)
    for cg in range(12):
        wt = wa[cg % 2]
        k.dma("sync", wt.t[:], w_ada_v[:, :, cg * 512:(cg + 1) * 512], w=[wt])
        ps = P.nps()
        for kc in range(8):
            P.mm(ps.t[:], sc_rep.t[:, kc, :], wt.t[:, kc, :], kc == 0, kc == 7, r=[sc_rep, wt], w=[ps])
        m, half = cg // 2, cg % 2
        if m < 2:
            dst = modA.t[:, m * D + half * 512: m * D + half * 512 + 512]
            dbuf = modA
        else:
            dst = P.modR.t[:, m - 2, half * 512: half * 512 + 512]
            dbuf = P.modR
        P.tt(dst, ps.t[:], bada.t[:, cg * 512:(cg + 1) * 512], ALU.add, r=[ps, bada], w=[dbuf])
    P.ts(modA.t[:, D:2 * D], modA.t[:, D:2 * D], 1.0, None, ALU.add, None, r=[modA], w=[modA])
    P.ts(P.modR.t[:, 2, :], P.modR.t[:, 2, :], 1.0, None, ALU.add, None, r=[P.modR], w=[P.modR])
    for m in range(2):
        for blk in range(8):
            ps = P.nps()
            P.tr(ps.t[:, 0:128], modA.t[:, m * D + blk * 128: m * D + (blk + 1) * 128], P.ident_f.t[:], r=[modA, P.ident_f], w=[ps])
            P.cp(P.modT.t[:, m, blk:blk + 1], ps.t[:, 0:1], r=[ps], w=[P.modT])
    if P.dbg:
        P.dump("sc", sc, sc.t[:], [128, 8])
        P.dump("screp", sc_rep, sc_rep.t[:, :, 0:2], [128, 8, 2])
        P.dump("modA", modA, modA.t[0:1, :], [1, 2 * D])
        P.dump("modR", P.modR, P.modR.t[0:1, :, :], [1, 4, D])
        P.dump("modT", P.modT, P.modT.t[:], [128, 2, 8])
    k.release(mark)
    if P.stage <= 0:
        return
    mark = k.mark()
    W1b = P.load_bf("W1b", [128, 8, 768], W1.rearrange("(k p) n -> p k n", p=128))
    wcqb = P.load_bf("wcqb", [128, 8, 256], wcq.rearrange("(k p) n -> p k n", p=128))
    xt = [k.sb(f"xt{i}", [128, 4, D], F32) for i in range(2)]
    uT = [k.sb(f"uT{i}", [128, 8, 512], BF16) for i in range(2)]
    posi = [k.sb(f"posi{i}", [128, 512], I32) for i in range(2)]
    ang = k.sb("ang", [128, 512], F32)
    ang2 = k.sb("ang2", [128, 512], F32)
    tmp = k.sb("tmp", [128, 512], F32)
    rr = k.sb("rr", [128, 512], F32)
    cosv = k.sb("cosv", [128, 512], F32)
    sinx = k.sb("sinx", [128, 512], F32)
    sq = [k.sb(f"sq{i}", [128, 512], BF16) for i in range(2)]
    rstd = k.sb("rstd", [128, 512], F32)
    t1 = k.sb("t1", [128, 512], F32)
    t2 = k.sb("t2", [128, 512], F32)
    hyst = [k.sb(f"hyst{i}", [128, 3, 512], BF16) for i in range(2)]

    def make_uT(i, xtile, utile):
        for kc in range(8):
            ps = P.nps()
            for s in range(4):
                P.tr(ps.t[:, s * 128:(s + 1) * 128], xtile.t[:, s, kc * 128:(kc + 1) * 128], P.ident_f.t[:],
                     r=[xtile, P.ident_f], w=[ps])
            P.act(utile.t[:, kc, :], ps.t[:], AF.Identity, r=[ps, P.modT], w=[utile],
                  scale=P.modT.t[:, 1, kc:kc + 1], bias=P.modT.t[:, 0, kc:kc + 1])

    def rope_tables(pos_tile, cos_out, sin_out, cos_buf, sin_buf):
        P.cp(ang.t[:], pos_tile.t[:], r=[pos_tile], w=[ang])
        P.ts(ang.t[:], ang.t[:], P.invf.t[:, 0:1], None, ALU.mult, None, r=[ang, P.invf], w=[ang])
        P.range_reduce(rr.t[:], ang.t[:], tmp.t[:], r=[ang], w=[tmp, rr])
        P.act(sin_out, rr.t[:], AF.Sin, r=[rr, P.sgn], w=[sin_buf], scale=P.sgn.t[:, 0:1])
        P.ts(ang2.t[:], ang.t[:], PI / 2, None, ALU.add, None, r=[ang], w=[ang2])
        P.range_reduce(rr.t[:], ang2.t[:], tmp.t[:], r=[ang2], w=[tmp, rr])
        P.act(cos_out, rr.t[:], AF.Sin, r=[rr], w=[cos_buf])

    def rms_T(ps_list, g_ap_list, out_aps, out_bufs, nfeat):
        ss = P.nps()
        for i, ps in enumerate(ps_list):
            P.act(sq[i].t[:], ps.t[:], AF.Square, r=[ps], w=[sq[i]])
            P.mm(ss.t[:], P.ones_b.t[:], sq[i].t[:], i == 0, i == len(ps_list) - 1, r=[P.ones_b, sq[i]], w=[ss])
        P.rsqrt(rstd.t[:], ss.t[:], 1.0 / nfeat, r=[ss], w=[rstd])
        for i, ps in enumerate(ps_list):
            P.stt(out_aps[i], ps.t[:], g_ap_list[i], rstd.t[:], ALU.mult, ALU.mult, r=[ps, rstd], w=[out_bufs[i]])

    xv = xb.rearrange("(t s p) d -> t p s d", p=128, s=4)
    for tt_ in range(16):
        xtile, utile, ptile = xt[tt_ % 2], uT[tt_ % 2], posi[tt_ % 2]
        k.dma("sync", xtile.t[:], xv[tt_], w=[xtile])
        k.dma("sync", ptile.t[:], posf[0:1, tt_ * 512:(tt_ + 1) * 512].to_broadcast([128, 512]), w=[ptile])
        make_uT(tt_, xtile, utile)
        pss = []
        for oc in range(6):
            ps = P.nps()
            for kc in range(8):
                P.mm(ps.t[:], W1b.t[:, kc, oc * 128:(oc + 1) * 128], utile.t[:, kc, :], kc == 0, kc == 7, r=[W1b, utile], w=[ps])
            pss.append(ps)
        sl = slice(tt_ * 512, (tt_ + 1) * 512)
        hs = hyst[tt_ % 2]
        for g in range(3):
            P.cp(hs.t[:, g, :], pss[3 + g].t[:], r=[pss[3 + g]], w=[hs], eng="scalar")
        k.dma("sync", P.hy_scr[:, :, sl], hs.t[:], r=[hs], w=[hy_l[tt_]])
        rope_tables(ptile, cosv.t[:], sinx.t[:], cosv, sinx)
        P.tt(t1.t[:], pss[1].t[:], cosv.t[:], ALU.mult, r=[pss[1], cosv], w=[t1])
        P.tt(t2.t[:], pss[2].t[:], sinx.t[:], ALU.mult, r=[pss[2], sinx], w=[t2])
        P.tt(P.krot.t[:, sl], t1.t[:], t2.t[:], ALU.add, r=[t1, t2], w=[krot_l[tt_]], eng="gpsimd")
        rms_T([pss[0]], [P.kvg.t[:, 0:1]], [P.ckvn.t[:, sl]], [ckvn_l[tt_]], 128)
    for tt_ in range(4):
        xtile, utile, ptile = xt[tt_ % 2], uT[tt_ % 2], posi[tt_ % 2]
        P.dyn_dma("sync", xtile.t[:], lambda base, tt_=tt_: xb[bass.ds(base, SO), :][tt_ * 512:(tt_ + 1) * 512, :].rearrange("(s p) d -> p s d", p=128), w=[xtile])
        P.dyn_dma("sync", ptile.t[:], lambda base, tt_=tt_: posf[0:1, bass.ds(base, SO)][:, tt_ * 512:(tt_ + 1) * 512].to_broadcast([128, 512]), w=[ptile])
        make_uT(tt_, xtile, utile)
        pss = []
        for oc in range(2):
            ps = P.nps()
            for kc in range(8):
                P.mm(ps.t[:], wcqb.t[:, kc, oc * 128:(oc + 1) * 128], utile.t[:, kc, :], kc == 0, kc == 7, r=[wcqb, utile], w=[ps])
            pss.append(ps)
        sl = slice(tt_ * 512, (tt_ + 1) * 512)
        rope_tables(ptile, P.cosO.t[:, sl], P.sinO.t[:, sl], cs_l[tt_], cs_l[tt_])
        rms_T(pss, [P.qg.t[:, 0:1], P.qg.t[:, 1:2]], [P.cqn.t[:, 0, sl], P.cqn.t[:, 1, sl]], [cqn_l[tt_], cqn_l[tt_]], 256)
    k.release(mark)
    if P.dbg and P.stage == 1:
        P.dump("ckvn", ckvn_l, P.ckvn.t[:], [128, S], BF16)
        P.dump("krot", krot_l, P.krot.t[:], [128, S], BF16)
        P.dump("cqn", cqn_l, P.cqn.t[:], [128, 2, SO], BF16)
        P.dump("cosO", cs_l, P.cosO.t[:], [128, SO])
        P.dump("sinO", cs_l, P.sinO.t[:], [128, SO])
        o = P.dout("dbg_hyT", [128, 3, S], BF16)
        k.dma("sync", o, P.hy_scr, r=hy_l, final=True)


def phase2(P):
    k = P.k
    wq = P.din("wq", [256, H * 2 * 128])
    wk = P.din("wk", [128, H * 128])
    wv = P.din("wv", [128, H * 64])
    mark = k.mark()
    wqb = P.load_bf("wqb", [128, 2, H * 2 * 128], wq.rearrange("(k p) n -> p k n", p=128))
    wkb = P.load_bf("wkb", [128, 1, H * 128], wk.rearrange("(k p) n -> p k n", p=128))
    wvb = P.load_bf("wvb", [128, 1, H * 64], wv.rearrange("(k p) n -> p k n", p=128))
    NS = 2
    KT = [k.sb(f"KT{i}", [128, S], BF16) for i in range(NS)]
    VX = [k.sb(f"VX{i}", [128, 64, 65], BF16) for i in range(NS)]
    QT = [k.sb(f"QT{i}", [128, SO], BF16) for i in range(NS)]
    for v in VX:
        P.memset(v.t[:, :, 64:65], 1.0, w=[v])
    PT = [k.sb(f"PT{i}", [128, 512], BF16) for i in range(4)]
    t1 = k.sb("at1", [128, 512], F32)
    t2 = k.sb("at2", [128, 512], F32)
    rc = k.sb("rc", [128, 4], F32)
    allps = P.PS
    P.PS = allps[0:4]
    acc = allps[4:8]
    pti = 0
    for h in range(H):
        kt_, vx, qt_ = KT[h % NS], VX[h % NS], QT[h % NS]
        for tg in range(16):
            sl = slice(tg * 512, (tg + 1) * 512)
            ps = P.nps()
            P.mm(ps.t[:], wkb.t[:, 0, h * 128:(h + 1) * 128], P.ckvn.t[:, sl], True, True, r=[wkb, P.ckvn_l[tg]], w=[ps])
            P.tt(kt_.t[:, sl], ps.t[:], P.krot.t[:, sl], ALU.add, r=[ps, P.krot_l[tg]], w=[kt_])
        for g8 in range(8):
            ps = P.nps()
            for i in range(8):
                kt = g8 * 8 + i
                P.mm(ps.t[:, i * 64:(i + 1) * 64], P.ckvn.t[:, kt * 128:(kt + 1) * 128], wvb.t[:, 0, h * 64:(h + 1) * 64], True, True,
                     r=[wvb, P.ckvn_l[kt // 4]], w=[ps])
            P.cp(vx.t[:, g8 * 8:(g8 + 1) * 8, 0:64], ps.t[:].rearrange("p (i d) -> p i d", i=8), r=[ps], w=[vx], eng="scalar")
        for tg in range(4):
            sl = slice(tg * 512, (tg + 1) * 512)
            psR, psP = P.nps(), P.nps()
            for kc in range(2):
                P.mm(psR.t[:], wqb.t[:, kc, (h * 2) * 128:(h * 2 + 1) * 128], P.cqn.t[:, kc, sl], kc == 0, kc == 1, r=[wqb, P.cqn_l[tg]], w=[psR])
            for kc in range(2):
                P.mm(psP.t[:], wqb.t[:, kc, (h * 2 + 1) * 128:(h * 2 + 2) * 128], P.cqn.t[:, kc, sl], kc == 0, kc == 1, r=[wqb, P.cqn_l[tg]], w=[psP])
            P.tt(t1.t[:], psR.t[:], P.cosO.t[:, sl], ALU.mult, r=[psR, P.cs_l[tg]], w=[t1])
            P.tt(t2.t[:], psP.t[:], P.sinO.t[:, sl], ALU.mult, r=[psP, P.cs_l[tg]], w=[t2])
            P.tt(qt_.t[:, sl], t1.t[:], t2.t[:], ALU.add, r=[t1, t2], w=[qt_], eng="gpsimd")
        LOOK = 2
        iters = [(qg, kt) for qg in range(4) for kt in range(64)]
        pend = []

        def emit_score(qg, kt):
            nonlocal pti
            ps = P.nps()
            P.mm(ps.t[:], kt_.t[:, kt * 128:(kt + 1) * 128], qt_.t[:, qg * 512:(qg + 1) * 512], True, True, r=[kt_, qt_], w=[ps])
            pt = PT[pti % len(PT)]
            pti += 1
            P.act(pt.t[:], ps.t[:], AF.Exp, r=[ps], w=[pt], scale=SCALE)
            return pt

        def emit_pv(qg, kt, pt):
            for q4 in range(4):
                P.mm(acc[q4].t[:, 0:65], pt.t[:, q4 * 128:(q4 + 1) * 128], vx.t[:, kt, :], kt == 0, kt == 63, r=[pt, vx], w=[acc[q4]])
            if kt == 63:
                for q4 in range(4):
                    tile_i = qg * 4 + q4
                    k.op("vector", lambda e, q4=q4: e.reciprocal(out=rc.t[:, q4:q4 + 1], in_=acc[q4].t[:, 64:65]), r=[acc[q4]], w=[rc])
                    P.ts(P.a_tok.t[:, tile_i, h * 64:(h + 1) * 64], acc[q4].t[:, 0:64], rc.t[:, q4:q4 + 1], None, ALU.mult, None,
                         r=[acc[q4], rc], w=[P.a_l[tile_i]])

        for idx, (qg, kt) in enumerate(iters):
            pend.append((qg, kt, emit_score(qg, kt)))
            if len(pend) > LOOK:
                emit_pv(*pend.pop(0))
        while pend:
            emit_pv(*pend.pop(0))
    P.PS = allps
    k.release(P.mark_attn)
    if P.dbg and P.stage == 2:
        P.dump("a_tok", P.a_l, P.a_tok.t[:], [128, 16, 512], BF16)


def build_all(P):
    phase01(P)
    if P.stage <= 1:
        return
    if os.environ.get("SKIP2", "0") != "1":
        phase2(P)
    else:
        P.k.release(P.mark_attn)
    if P.stage <= 2:
        return
    phase3(P)
    if P.stage <= 3:
        return
    phase4(P)
    if P.stage <= 4:
        return
    phase5(P)
    phase6(P)


def phase3(P):
    k = P.k
    L1 = float(S - 1)
    cw = P.din("cw", [128, 9]); cb = P.din("cb", [128, 3])
    featsT = P.din("featsT", [33, NFFT])
    fw1 = P.din("fw1", [33, 64]); fb1 = P.din("fb1", [64, 1]); ffreq = P.din("ffreq", [64, 1])
    fw2 = P.din("fw2", [64, 64]); fb2 = P.din("fb2", [64, 1])
    fw3 = P.din("fw3", [64, 512]); fb3 = P.din("fb3", [1, 512]); fdec = P.din("fdec", [1, 512])
    hbias = P.din("hbias", [1, 256])
    e1s = P.din("e1s", [128, 1]); n2row = P.din("n2row", [128, 128])
    cn = {n: P.din(n, shp) for n, shp in [("F1", [128, 256]), ("C2", [128, 128]), ("S2", [128, 128]), ("S2n", [128, 128]),
                                           ("G1", [128, 256]), ("G2", [128, 256]), ("TC", [128, 128]), ("TS", [128, 128]),
                                           ("TSn", [128, 128]), ("CI", [128, 64]), ("SIn", [128, 64])]}
    P.hy_bounce = [P.dscr(f"hy_bounce{g}", [32, S], F32) for g in range(4)]
    P.hyb_l = [k.buf(f"hy_bounce{g}") for g in range(4)]
    P.hy_all = [P.dscr(f"hy_all{g}", [4 * 32, S], F32) for g in range(4)]
    P.hya_l = [k.buf(f"hy_all{g}") for g in range(4)]
    L_scr = [P.dscr(f"L_scr{g}", [64, 128, 128], BF16) for g in range(3)]
    L_lab = [k.buf(f"L_scr{g}") for g in range(3)]
    z2_scr = P.dscr("z2_scr", [64, 128, 128], BF16)
    z2_lab = [k.buf(f"z2_{g}") for g in range(4)]
    mark = k.mark()
    def ldc_bf(name, shape):
        st = P.load(name + "_f", shape, cn[name])
        b = k.sb(name + "_b", shape, BF16)
        P.cp(b.t[:], st.t[:], r=[st], w=[b])
        return b
    TC = P.load("TC", [128, 128], cn["TC"]); TS = P.load("TS", [128, 128], cn["TS"]); TSn = P.load("TSn", [128, 128], cn["TSn"])
    mk2 = k.mark()
    F1b = k.sb("F1b", [128, 256], BF16); C2b = k.sb("C2b", [128, 128], BF16); S2b = k.sb("S2b", [128, 128], BF16)
    S2nb = k.sb("S2nb", [128, 128], BF16); G1b = k.sb("G1b", [128, 256], BF16); G2b = k.sb("G2b", [128, 256], BF16)
    CIb = k.sb("CIb", [128, 64], BF16); SInb = k.sb("SInb", [128, 64], BF16)
    stg = k.sb("cstg", [128, 256], F32)
    for nm, b, w_ in [("F1", F1b, 256), ("C2", C2b, 128), ("S2", S2b, 128), ("S2n", S2nb, 128), ("G1", G1b, 256), ("G2", G2b, 256),
                      ("CI", CIb, 64), ("SIn", SInb, 64)]:
        k.dma("sync", stg.t[:, 0:w_], cn[nm], w=[stg])
        P.cp(b.t[:], stg.t[:, 0:w_], r=[stg], w=[b])
    cwt = P.load("cwt", [128, 9], cw); cbt = P.load("cbt", [128, 3], cb)
    e1st = P.load("e1st", [128, 1], e1s); n2r = P.load("n2r", [128, 128], n2row)
    b3bc = P.load("b3bc", [128, 512], fb3.to_broadcast([128, 512]))
    decbc = P.load("decbc", [128, 512], fdec.to_broadcast([128, 512]))
    dneg = k.sb("dneg", [128, 512], F32)
    P.ts(dneg.t[:], decbc.t[:], -1.0, None, ALU.mult, None, r=[decbc], w=[dneg])
    P.tt(decbc.t[:], decbc.t[:], dneg.t[:], ALU.max, r=[decbc, dneg], w=[decbc])
    hbbc = P.load("hbbc", [128, 256], hbias.to_broadcast([128, 256]))
    w3st = P.load("w3st", [64, 512], fw3)
    w3b = k.sb("w3b", [64, 512], BF16)
    P.cp(w3b.t[:], w3st.t[:], r=[w3st], w=[w3b])
    h2T = k.sb("h2T", [64, NFFT], BF16)
    mk3 = k.mark()
    w1t = P.load("w1t", [33, 64], fw1); w2t = P.load("w2t", [64, 64], fw2)
    b1t = P.load("b1t", [64, 1], fb1); b2t = P.load("b2t", [64, 1], fb2); frt = P.load("frt", [64, 1], ffreq)
    fb1f = k.sb("fb1f", [64, 1], F32); fb2f = k.sb("fb2f", [64, 1], F32)
    P.tt(fb1f.t[:], b1t.t[:], frt.t[:], ALU.mult, r=[b1t, frt], w=[fb1f])
    P.tt(fb2f.t[:], b2t.t[:], frt.t[:], ALU.mult, r=[b2t, frt], w=[fb2f])
    fe = [k.sb(f"fe{i}", [33, 2048], F32) for i in range(2)]
    arg = k.sb("farg", [64, 512], F32); ftmp = k.sb("ftmp", [64, 512], F32); frr = k.sb("frr", [64, 512], F32)
    h1 = k.sb("fh1", [64, 512], F32)
    arg2 = k.sb("farg2", [64, 512], F32); ftmp2 = k.sb("ftmp2", [64, 512], F32); frr2 = k.sb("frr2", [64, 512], F32)
    for ch in range(8):
        f = fe[ch % 2]
        k.dma("sync", f.t[:], featsT[:, ch * 2048:(ch + 1) * 2048], w=[f])
        for c5 in range(4):
            col = ch * 2048 + c5 * 512
            ps = P.nps()
            P.mm(ps.t[0:64, :], w1t.t[:, :], f.t[:, c5 * 512:(c5 + 1) * 512], True, True, r=[w1t, f], w=[ps])
            P.ts(arg.t[:], ps.t[0:64, :], frt.t[:, 0:1], fb1f.t[:, 0:1], ALU.mult, ALU.add, r=[ps, frt, fb1f], w=[arg])
            P.range_reduce(frr.t[:], arg.t[:], ftmp.t[:], r=[arg], w=[ftmp, frr])
            P.act(h1.t[:], frr.t[:], AF.Sin, r=[frr], w=[h1])
            ps2 = P.nps()
            P.mm(ps2.t[0:64, :], w2t.t[:, :], h1.t[:, :], True, True, r=[w2t, h1], w=[ps2])
            P.ts(arg2.t[:], ps2.t[0:64, :], frt.t[:, 0:1], fb2f.t[:, 0:1], ALU.mult, ALU.add, r=[ps2, frt, fb2f], w=[arg2])
            P.range_reduce(frr2.t[:], arg2.t[:], ftmp2.t[:], r=[arg2], w=[ftmp2, frr2])
            P.act(h2T.t[:, col:col + 512], frr2.t[:], AF.Sin, r=[frr2], w=[h2T])
    k.release(mk3)
    mk3 = k.mark()
    raw = k.sb("raw", [128, S + 2], BF16)
    uc = k.sb("uc", [128, S], BF16)
    accb = k.sb("accb", [128, 2048], F32)
    Lst = k.sb("Lst", [64, 128, 128], BF16)
    for g in range(3):
        P.memset(raw.t[:, 0:1], 0.0, w=[raw])
        P.memset(raw.t[:, S + 1:S + 2], 0.0, w=[raw])
        k.dma("sync", raw.t[:, 1:S + 1], P.hy_scr[:, g, :], r=P.hy_l, w=[raw])
        for c4 in range(4):
            c0 = c4 * 2048
            P.ts(accb.t[:], raw.t[:, c0 + 1:c0 + 2049], cwt.t[:, g * 3 + 1:g * 3 + 2], cbt.t[:, g:g + 1], ALU.mult, ALU.add, r=[raw, cwt, cbt], w=[accb])
            P.stt(accb.t[:], raw.t[:, c0:c0 + 2048], cwt.t[:, g * 3:g * 3 + 1], accb.t[:], ALU.mult, ALU.add, r=[raw, cwt, accb], w=[accb])
            P.stt(uc.t[:, c0:c0 + 2048], raw.t[:, c0 + 2:c0 + 2050], cwt.t[:, g * 3 + 2:g * 3 + 3], accb.t[:], ALU.mult, ALU.add, r=[raw, cwt, accb], w=[uc])
        ucv = uc.t[:, :].rearrange("p (a b) -> p b a", b=128)
        for b8 in range(16):
            ps = P.nps()
            psb = ps.t[:].bitcast(BF16)
            for i in range(8):
                n2 = b8 * 8 + i
                P.tr(psb[0:64, i * 128:(i + 1) * 128], ucv[:, n2, :], P.ident_b.t[:], r=[uc, P.ident_b], w=[ps])
            P.cp(Lst.t[:, :, b8 * 8:(b8 + 1) * 8].rearrange("p c i -> p i c"), psb[0:64, :].rearrange("p (i c) -> p i c", c=128),
                 r=[ps], w=[Lst], eng=("scalar" if b8 % 2 else "vector"))
        k.dma("sync", L_scr[g], Lst.t[:], r=[Lst], w=[L_lab[g]])
    k.release(mk3)
    hraw = k.sb("hraw", [128, 32, 128], F32)
    E2 = k.sb("E2", [128, 32, 128], F32)
    hL = k.sb("hL", [128, 32, 128], BF16)
    absd = k.sb("absd", [128, 32], F32); dsel = k.sb("dsel", [128, 32], F32); E1 = k.sb("E1", [128, 32], F32)
    Hre = k.sb("Hre", [128, 32, 128], BF16); Him = k.sb("Him", [128, 32, 128], BF16)
    Are = k.sb("Are", [128, 32, 128], BF16); Aim = k.sb("Aim", [128, 32, 128], BF16)
    Yre = k.sb("Yre", [128, 32, 128], BF16); Yim = k.sb("Yim", [128, 32, 128], BF16)
    zg = k.sb("zg", [64, 32, 128], BF16); xg = k.sb("xg", [64, 32, 128], BF16)
    z2g = hL; outg = hraw
    MS = [[k.sb(f"m{i}_{q}", [128, 512], F32) for i in range(4)] for q in range(2)]
    msi = [0]

    def next_ms():
        msi[0] += 1
        return MS[msi[0] % len(MS)]
    h2v = h2T.t[:, :].rearrange("p (a b) -> p b a", b=128)

    def twiddle(psA, dre, dim, c0, inverse):
        m1, m2, m3, m4 = next_ms()
        v = psA.t[:].rearrange("p (c r k) -> p c r k", c=2, r=2)
        are, aim = v[:, :, 0, :], v[:, :, 1, :]
        tc = TC.t[:].unsqueeze(1).to_broadcast([128, 2, 128])
        tsn = TSn.t[:].unsqueeze(1).to_broadcast([128, 2, 128])
        tsp = TS.t[:].unsqueeze(1).to_broadcast([128, 2, 128])
        a, b, c_, d = (m1.t[:, 0:256].rearrange("p (c k) -> p c k", c=2), m2.t[:, 0:256].rearrange("p (c k) -> p c k", c=2),
                       m3.t[:, 0:256].rearrange("p (c k) -> p c k", c=2), m4.t[:, 0:256].rearrange("p (c k) -> p c k", c=2))
        P.tt(a, are, tc, ALU.mult, r=[psA, TC], w=[m1])
        P.tt(b, aim, tsn if inverse else tsp, ALU.mult, r=[psA, TS, TSn], w=[m2])
        P.tt(dre.t[:, c0:c0 + 2, :], a, b, ALU.add, r=[m1, m2], w=[dre], eng="gpsimd")
        P.tt(c_, aim, tc, ALU.mult, r=[psA, TC], w=[m3])
        P.tt(d, are, tsp if inverse else tsn, ALU.mult, r=[psA, TS, TSn], w=[m4])
        P.tt(dim.t[:, c0:c0 + 2, :], c_, d, ALU.add, r=[m3, m4], w=[dim], eng="gpsimd")

    def fwd_stage1(src, Kn):
        for c2 in range(16):
            ps = P.nps()
            for i in range(2):
                c = c2 * 2 + i
                P.mm(ps.t[:, i * 256:(i + 1) * 256], src.t[0:Kn, c, :], F1b.t[0:Kn, :], True, True, r=[src, F1b], w=[ps])
            twiddle(ps, Are, Aim, c2 * 2, False)

    for order in range(2):
        for grp in range(4):
            col0 = (order * 4 + grp) * 64
            P.cp(absd.t[0:64, :], decbc.t[0:64, col0:col0 + 32], r=[decbc], w=[absd])
            P.cp(absd.t[64:128, :], decbc.t[64:128, col0 + 32:col0 + 64], r=[decbc], w=[absd])
            P.ts(dsel.t[0:64, :], absd.t[0:64, :], -1.0 / L1, None, ALU.mult, None, r=[absd], w=[dsel])
            P.ts(dsel.t[64:128, :], absd.t[64:128, :], 1.0 / L1, None, ALU.mult, None, r=[absd], w=[dsel])
            P.act(E1.t[:], absd.t[:], AF.Exp, r=[absd, e1st], w=[E1], scale=e1st.t[:, 0:1])
            P.tt(E2.t[:], n2r.t[:].unsqueeze(1).to_broadcast([128, 32, 128]), dsel.t[:].unsqueeze(2).to_broadcast([128, 32, 128]),
                 ALU.mult, r=[n2r, dsel], w=[E2], eng="gpsimd")
            P.act(E2.t[:], E2.t[:], AF.Exp, r=[E2], w=[E2])
            for b8 in range(16):
                ps = P.nps()
                for i in range(8):
                    n2 = b8 * 8 + i
                    P.mm(ps.t[:, i * 64:(i + 1) * 64], h2v[:, n2, :], w3b.t[:, col0:col0 + 64], True, True, r=[h2T, w3b], w=[ps])
                pv = ps.t[:].rearrange("p (i c) -> p c i", c=64)
                P.tt(hraw.t[0:64, :, b8 * 8:(b8 + 1) * 8], pv[0:64, 0:32, :],
                     b3bc.t[0:64, col0:col0 + 32].unsqueeze(2).to_broadcast([64, 32, 8]), ALU.add, r=[ps, b3bc], w=[hraw])
                P.tt(hraw.t[64:128, :, b8 * 8:(b8 + 1) * 8], pv[64:128, 32:64, :],
                     b3bc.t[64:128, col0 + 32:col0 + 64].unsqueeze(2).to_broadcast([64, 32, 8]), ALU.add, r=[ps, b3bc], w=[hraw])
            P.tt(hraw.t[:], hraw.t[:], E1.t[:].unsqueeze(2).to_broadcast([128, 32, 128]), ALU.mult, r=[hraw, E1], w=[hraw])
            P.tt(hL.t[:], hraw.t[:], E2.t[:], ALU.mult, r=[hraw, E2], w=[hL], eng="gpsimd")
            P.memset(hL.t[64:65, :, 0:1], 0.0, w=[hL])
            fwd_stage1(hL, 128)
            for c8 in range(8):
                sl = slice(c8 * 4, c8 * 4 + 4)
                pre, pim = P.nps(), P.nps()
                ar = Are.t[:, sl, :].rearrange("p c k -> p (c k)"); ai = Aim.t[:, sl, :].rearrange("p c k -> p (c k)")
                P.mm(pre.t[:], C2b.t[:], ar, True, False, r=[C2b, Are], w=[pre])
                P.mm(pre.t[:], S2b.t[:], ai, False, True, r=[S2b, Aim], w=[pre])
                P.mm(pim.t[:], C2b.t[:], ai, True, False, r=[C2b, Aim], w=[pim])
                P.mm(pim.t[:], S2nb.t[:], ar, False, True, r=[S2nb, Are], w=[pim])
                P.cp(Hre.t[:, sl, :].rearrange("p c k -> p (c k)"), pre.t[:], r=[pre], w=[Hre], eng="scalar")
                P.cp(Him.t[:, sl, :].rearrange("p c k -> p (c k)"), pim.t[:], r=[pim], w=[Him], eng="scalar")
            src_ap = (L_scr[0] if order == 0 else z2_scr)[:, grp * 32:(grp + 1) * 32, :]
            k.dma("sync", zg.t[:], src_ap, r=([L_lab[0]] if order == 0 else [z2_lab[grp]]), w=[zg])
            k.dma("sync", xg.t[:], L_scr[1 + order][:, grp * 32:(grp + 1) * 32, :], r=[L_lab[1 + order]], w=[xg])
            fwd_stage1(zg, 64)
            for c8 in range(8):
                sl = slice(c8 * 4, c8 * 4 + 4)
                pre, pim = P.nps(), P.nps()
                ar = Are.t[:, sl, :].rearrange("p c k -> p (c k)"); ai = Aim.t[:, sl, :].rearrange("p c k -> p (c k)")
                P.mm(pre.t[:], C2b.t[:], ar, True, False, r=[C2b, Are], w=[pre])
                P.mm(pre.t[:], S2b.t[:], ai, False, True, r=[S2b, Aim], w=[pre])
                P.mm(pim.t[:], C2b.t[:], ai, True, False, r=[C2b, Aim], w=[pim])
                P.mm(pim.t[:], S2nb.t[:], ar, False, True, r=[S2nb, Are], w=[pim])
                hr = Hre.t[:, sl, :].rearrange("p c k -> p (c k)"); hi = Him.t[:, sl, :].rearrange("p c k -> p (c k)")
                m1, m2, m3, m4 = next_ms()
                P.tt(m1.t[:], pre.t[:], hr, ALU.mult, r=[pre, Hre], w=[m1])
                P.tt(m2.t[:], pim.t[:], hi, ALU.mult, r=[pim, Him], w=[m2])
                P.tt(Yre.t[:, sl, :].rearrange("p c k -> p (c k)"), m1.t[:], m2.t[:], ALU.subtract, r=[m1, m2], w=[Yre], eng="gpsimd")
                P.tt(m3.t[:], pre.t[:], hi, ALU.mult, r=[pre, Him], w=[m3])
                P.tt(m4.t[:], pim.t[:], hr, ALU.mult, r=[pim, Hre], w=[m4])
                P.tt(Yim.t[:, sl, :].rearrange("p c k -> p (c k)"), m3.t[:], m4.t[:], ALU.add, r=[m3, m4], w=[Yim], eng="gpsimd")
            for c2 in range(16):
                ps = P.nps()
                for i in range(2):
                    c = c2 * 2 + i
                    P.mm(ps.t[:, i * 256:(i + 1) * 256], Yre.t[:, c, :], G1b.t[:], True, False, r=[Yre, G1b], w=[ps])
                    P.mm(ps.t[:, i * 256:(i + 1) * 256], Yim.t[:, c, :], G2b.t[:], False, True, r=[Yim, G2b], w=[ps])
                twiddle(ps, Are, Aim, c2 * 2, True)
            hb0 = order * 128 + grp * 32
            for c8 in range(8):
                sl = slice(c8 * 4, c8 * 4 + 4)
                py = P.nps()
                br = Are.t[:, sl, :].rearrange("p c k -> p (c k)"); bi = Aim.t[:, sl, :].rearrange("p c k -> p (c k)")
                P.mm(py.t[0:64, :], CIb.t[:], br, True, False, r=[CIb, Are], w=[py])
                P.mm(py.t[0:64, :], SInb.t[:], bi, False, True, r=[SInb, Aim], w=[py])
                m1 = next_ms()[0]
                t_ = m1.t[0:64, :].rearrange("p (c k) -> p c k", c=4)
                P.tt(t_, zg.t[:, sl, :], hbbc.t[0:64, hb0 + c8 * 4:hb0 + c8 * 4 + 4].unsqueeze(2).to_broadcast([64, 4, 128]), ALU.mult,
                     r=[zg, hbbc], w=[m1])
                P.tt(m1.t[0:64, :], m1.t[0:64, :], py.t[0:64, :], ALU.add, r=[m1, py], w=[m1])
                if order == 0:
                    P.tt(z2g.t[0:64, sl, :], t_, xg.t[:, sl, :], ALU.mult, r=[m1, xg], w=[z2g], eng="gpsimd")
                else:
                    P.tt(outg.t[0:64, sl, :], t_, xg.t[:, sl, :], ALU.mult, r=[m1, xg], w=[outg], eng="gpsimd")
            if order == 0:
                k.dma("sync", z2_scr[:, grp * 32:(grp + 1) * 32, :], z2g.t[0:64, :, :], r=[z2g], w=[z2_lab[grp]])
            else:
                k.dma("sync", P.hy_bounce[grp].rearrange("c (a b) -> a c b", b=128), outg.t[0:64, :, :], r=[outg], w=[P.hyb_l[grp]])
                k.collective("AllGather", ALU.bypass, [[0, 1, 2, 3], [4, 5, 6, 7]], ins=[P.hy_bounce[grp]], outs=[P.hy_all[grp]],
                             r=[P.hyb_l[grp]], w=[P.hya_l[grp]])
    k.release(mark)
    if P.dbg and P.stage == 3:
        o2 = P.dout("dbg_L0", [64, 128, 128], BF16)
        k.dma("sync", o2, L_scr[0], r=[L_lab[0]], final=True)


def phase4(P):
    k = P.k
    xb = P.inp["xb"]
    ag = P.din("ag", [1, 512]); hg = P.din("hg", [128, 4]); w_o = P.din("w_o", [D, D])
    ln1g = P.din("ln1g", [1, D]); ln1b = P.din("ln1b", [1, D]); wr = P.din("wr", [D, 16])
    hy_all = P.hy_all; hya_l = P.hya_l
    P.x1_scr = P.dscr("x1_scr", [SO, D], F32); P.x1_l = [k.buf(f"x1_{t}") for t in range(16)]
    P.u2_bounce = [P.dscr(f"u2_bounce{c}", [512, D], BF16) for c in range(4)]; u2b_l = [k.buf(f"u2_bounce{c}") for c in range(4)]
    P.aff_bounce = P.dscr("aff_bounce", [SO, 16], F32); affb_l = k.buf("aff_bounce")
    P.u2_all = [P.dscr(f"u2_all{c}", [4 * 512, D], BF16) for c in range(4)]; P.u2a_l = [k.buf(f"u2_all{c}") for c in range(4)]
    P.aff_all = P.dscr("aff_all", [S, 16], F32); P.affa_l = k.buf("aff_all")
    GR = [[0, 1, 2, 3], [4, 5, 6, 7]]

    P.aff_own = k.sb("aff_own", [128, 16, 16], F32)
    P.affo_l = k.buf("aff_own_l")
    mark = k.mark()
    wob = P.load_bf("wob", [128, 8, D], w_o.rearrange("(k p) n -> p k n", p=128))
    ln1g_bc = P.load("ln1g_bc", [128, D], ln1g.to_broadcast([128, D]))
    ln1b_bc = P.load("ln1b_bc", [128, D], ln1b.to_broadcast([128, D]))
    ag_bc = P.load("ag_bc", [128, 512], ag.to_broadcast([128, 512]))
    hgt = P.load("hgt", [128, 4], hg)
    wrt = P.load("wrt", [128, 8, 16], wr.rearrange("(k p) n -> p k n", p=128))
    hyo = k.sb("hyo", [128, 4, 512], F32)
    hnT = k.sb("hnT", [128, 4, 512], BF16)
    sqh = [k.sb(f"sqh{i}", [128, 512], BF16) for i in range(2)]
    rstdh = k.sb("rstdh", [128, 512], F32)
    junk = k.sb("junk", [128, D], F32)
    st = k.sb("st4", [128, 8], F32)
    an = k.sb("an", [128, 512], BF16)
    anT = k.sb("anT", [128, 4, 128], BF16)
    xo = [k.sb(f"xo{i}", [128, D], F32) for i in range(2)]
    v = k.sb("v4", [128, D], F32)
    x1 = [k.sb(f"x1t{i}", [128, D], F32) for i in range(2)]
    u2 = k.sb("u2t", [128, D], F32)
    u2b = [k.sb(f"u2b{i}", [128, D], BF16) for i in range(2)]
    u2T = k.sb("u2T", [128, 8, 128], F32)
    ex = k.sb("ex", [128, 16], F32)
    for tg in range(4):
        for g4 in range(4):
            P.dyn_dma("sync", hyo.t[g4 * 32:(g4 + 1) * 32, :, :],
                      lambda base, tg=tg, g4=g4: hy_all[g4].rearrange("(k c) t -> c k t", c=32)[:, :, bass.ds(base, SO)][:, :, tg * 512:(tg + 1) * 512],
                      r=[hya_l[g4]], w=[hyo])
        ss = P.nps()
        for kc in range(4):
            sq = sqh[kc % 2]
            P.act(sq.t[:], hyo.t[:, kc, :], AF.Square, r=[hyo], w=[sq])
            P.mm(ss.t[:], P.ones_b.t[:], sq.t[:], kc == 0, kc == 3, r=[P.ones_b, sq], w=[ss])
        P.rsqrt(rstdh.t[:], ss.t[:], 1.0 / 512, r=[ss], w=[rstdh])
        for kc in range(4):
            P.stt(hnT.t[:, kc, :], hyo.t[:, kc, :], hgt.t[:, kc:kc + 1], rstdh.t[:], ALU.mult, ALU.mult, r=[hyo, hgt, rstdh], w=[hnT])
        for t4 in range(4):
            t = tg * 4 + t4
            xt_ = xo[t % 2]
            P.dyn_dma("sync", xt_.t[:], lambda base, t=t: xb[bass.ds(base, SO), :][t * 128:(t + 1) * 128, :], w=[xt_])
            P.act(junk.t[:, 0:512], P.a_tok.t[:, t, :], AF.Square, r=[P.a_l[t]], w=[junk, st], accum_out=st.t[:, 0:1])
            P.rsqrt(st.t[:, 1:2], st.t[:, 0:1], 1.0 / 512, r=[st], w=[st])
            P.stt(an.t[:], P.a_tok.t[:, t, :], st.t[:, 1:2], ag_bc.t[:], ALU.mult, ALU.mult, r=[P.a_l[t], st, ag_bc], w=[an])
            ps = P.nps()
            psb = ps.t[:].bitcast(BF16)
            for kc in range(4):
                P.tr(psb[:, kc * 128:(kc + 1) * 128], an.t[:, kc * 128:(kc + 1) * 128], P.ident_b.t[:], r=[an, P.ident_b], w=[ps])
            P.cp(anT.t[:].rearrange("p k t -> p (k t)"), psb[:, 0:512], r=[ps], w=[anT], eng="scalar")
            for half in range(2):
                hs = slice(half * 512, (half + 1) * 512)
                pm = P.nps()
                for kc in range(4):
                    P.mm(pm.t[:], anT.t[:, kc, :], wob.t[:, kc, hs], kc == 0, False, r=[anT, wob], w=[pm])
                for kc in range(4):
                    P.mm(pm.t[:], hnT.t[:, kc, t4 * 128:(t4 + 1) * 128], wob.t[:, 4 + kc, hs], False, kc == 3, r=[hnT, wob], w=[pm])
                P.tt(junk.t[:, hs], pm.t[:], P.modR.t[:, 0, hs], ALU.mult, r=[pm, P.modR], w=[junk])
                P.stt(v.t[:, hs], xt_.t[:, hs], ALPHA, junk.t[:, hs], ALU.mult, ALU.add, r=[xt_, junk], w=[v])
            x1t = x1[t % 2]
            P.layer_norm(x1t, v, ln1g_bc, ln1b_bc, st, junk)
            k.dma("sync", P.x1_scr[t * 128:(t + 1) * 128, :], x1t.t[:], r=[x1t], w=[P.x1_l[t]])
            P.tt(u2.t[:], x1t.t[:], P.modR.t[:, 2, :], ALU.mult, r=[x1t, P.modR], w=[u2])
            P.tt(u2.t[:], u2.t[:], P.modR.t[:, 1, :], ALU.add, r=[u2, P.modR], w=[u2], eng="gpsimd")
            ub = u2b[t % 2]
            P.cp(ub.t[:], u2.t[:], r=[u2], w=[ub], eng="scalar")
            k.dma("sync", P.u2_bounce[tg][t4 * 128:(t4 + 1) * 128, :], ub.t[:], r=[ub], w=[u2b_l[tg]])
            for hb in range(2):
                ps = P.nps()
                for i in range(4):
                    kc = hb * 4 + i
                    P.tr(ps.t[:, i * 128:(i + 1) * 128], u2.t[:, kc * 128:(kc + 1) * 128], P.ident_f.t[:], r=[u2, P.ident_f], w=[ps])
                P.cp(u2T.t[:, hb * 4:(hb + 1) * 4, :].rearrange("p k t -> p (k t)"), ps.t[:], r=[ps], w=[u2T], eng=("scalar" if hb else "vector"))
            pl = P.nps()
            for kc in range(8):
                P.mm(pl.t[:, 0:16], u2T.t[:, kc, :], wrt.t[:, kc, :], kc == 0, kc == 7, r=[u2T, wrt], w=[pl])
            k.op("vector", lambda e, pl=pl: e.tensor_reduce(out=st.t[:, 4:5], in_=pl.t[:, 0:16], axis=AX.X, op=ALU.max), r=[pl], w=[st])
            P.ts(st.t[:, 5:6], st.t[:, 4:5], -1.0, None, ALU.mult, None, r=[st], w=[st])
            P.act(ex.t[:], pl.t[:, 0:16], AF.Exp, r=[pl, st], w=[ex, st], bias=st.t[:, 5:6], accum_out=st.t[:, 6:7])
            k.op("vector", lambda e: e.reciprocal(out=st.t[:, 7:8], in_=st.t[:, 6:7]), r=[st], w=[st])
            P.ts(P.aff_own.t[:, t, :], ex.t[:], st.t[:, 7:8], None, ALU.mult, None, r=[ex, st], w=[P.affo_l])
            k.dma("sync", P.aff_bounce[t * 128:(t + 1) * 128, :], P.aff_own.t[:, t, :], r=[P.affo_l], w=[affb_l])
            if t4 == 3:
                k.collective("AllGather", ALU.bypass, GR, ins=[P.u2_bounce[tg]], outs=[P.u2_all[tg]], r=[u2b_l[tg]], w=[P.u2a_l[tg]])
    k.collective("AllGather", ALU.bypass, GR, ins=[P.aff_bounce], outs=[P.aff_all], r=[affb_l], w=[P.affa_l])
    k.release(mark)
    if P.dbg and P.stage == 4:
        o = P.dout("dbg_x1", [SO, D]); k.dma("sync", o, P.x1_scr, r=P.x1_l, final=True)
        o = P.dout("dbg_aff", [S, 16]); k.dma("sync", o, P.aff_all, r=[P.affa_l], final=True)
        pass


def _layer_norm(P, out, v, g_bc, b_bc, st, junk):
    k = P.k
    k.op("vector", lambda e: e.tensor_reduce(out=st.t[:, 0:1], in_=v.t[:], axis=AX.X, op=ALU.add), r=[v], w=[st])
    P.ts(st.t[:, 1:2], st.t[:, 0:1], -1.0 / D, None, ALU.mult, None, r=[st], w=[st])
    P.ts(v.t[:], v.t[:], st.t[:, 1:2], None, ALU.add, None, r=[v, st], w=[v])
    P.act(junk.t[:], v.t[:], AF.Square, r=[v], w=[junk, st], accum_out=st.t[:, 2:3])
    P.rsqrt(st.t[:, 3:4], st.t[:, 2:3], 1.0 / D, r=[st], w=[st])
    P.stt(out.t[:], v.t[:], st.t[:, 3:4], g_bc.t[:], ALU.mult, ALU.mult, r=[v, st, g_bc], w=[out])
    P.tt(out.t[:], out.t[:], b_bc.t[:], ALU.add, r=[out, b_bc], w=[out], eng="gpsimd")


Prog.layer_norm = _layer_norm

BIG = float(1 << 20)
CAP = 1024


def phase5(P):
    k = P.k
    GR = [[0, 1, 2, 3], [4, 5, 6, 7]]
    wg = P.din("wg", [4, D, D]); wu = P.din("wu", [4, D, D]); wd = P.din("wd", [4, D, D])
    P.pos_scr = P.dscr("pos_scr", [S, 16], I32); P.pos_l = k.buf("pos_scr")
    xe_dram = [P.dscr(f"xe_dram{i}", [CAP + 128, D], BF16) for i in range(4)]; xe_l = [k.buf(f"xe{i}") for i in range(4)]
    ye_bounce = [[P.dscr(f"ye_b{i}_{h}", [512, D], BF16) for h in range(2)] for i in range(4)]
    ye_allc = [[P.dscr(f"ye_allc{i}_{h}", [4 * 512, D], BF16) for h in range(2)] for i in range(4)]
    P.ye_full = [P.dscr(f"ye_full{i}", [4 * CAP, D], BF16) for i in range(4)]
    yeb_l = [[k.buf(f"yeb{i}{h}") for h in range(2)] for i in range(4)]
    yec_l = [[k.buf(f"yec{i}{h}") for h in range(2)] for i in range(4)]
    P.yea_l = [k.buf(f"yea{i}") for i in range(4)]
    mark = k.mark()
    affT = k.sb("affT", [128, 64, 16], F32)
    k.dma("sync", affT.t[:], P.aff_all.rearrange("(f p) e -> p f e", p=128), r=[P.affa_l], w=[affT])
    A = k.sb("A5", [128, 16, 64], F32)
    P.cp(A.t[:], affT.t[:].rearrange("p f e -> p e f"), r=[affT], w=[A])
    ones_f = k.sb("ones_f", [128, 128], F32); P.memset(ones_f.t[:], 1.0, w=[ones_f])
    U = k.sb("U5", [128, 128], F32)
    P.memset(U.t[:], 1.0, w=[U], eng="gpsimd")
    k.op("gpsimd", lambda e: e.affine_select(out=U.t[:], in_=U.t[:], pattern=[[1, 128]], compare_op=ALU.is_gt, fill=0.0,
                                             base=0, channel_multiplier=-1), r=[U], w=[U])
    Ub = k.sb("Ub", [128, 128], BF16); P.cp(Ub.t[:], U.t[:], r=[U], w=[Ub])
    lo = k.sb("lo", [128, 16], F32); hi = k.sb("hi", [128, 16], F32); mid = k.sb("mid", [128, 16], F32)
    cnt = k.sb("cnt", [128, 16], F32); ge = k.sb("ge", [128, 16], F32); d1 = k.sb("d1", [128, 16], F32)
    cmp = k.sb("cmp", [128, 16, 64], F32)
    P.memset(lo.t[:], 0.0, w=[lo]); P.memset(hi.t[:], 1.0, w=[hi])
    for it in range(30):
        P.tt(mid.t[:], lo.t[:], hi.t[:], ALU.add, r=[lo, hi], w=[mid])
        P.ts(mid.t[:], mid.t[:], 0.5, None, ALU.mult, None, r=[mid], w=[mid])
        P.tt(cmp.t[:], A.t[:], mid.t[:].unsqueeze(2).to_broadcast([128, 16, 64]), ALU.is_ge, r=[A, mid], w=[cmp])
        k.op("vector", lambda e: e.tensor_reduce(out=cnt.t[:], in_=cmp.t[:], axis=AX.X, op=ALU.add), r=[cmp], w=[cnt])
        pt = P.nps()
        P.mm(pt.t[:, 0:16], ones_f.t[:], cnt.t[:], True, True, r=[ones_f, cnt], w=[pt])
        P.ts(ge.t[:], pt.t[:, 0:16], CAP - 0.5, None, ALU.is_ge, None, r=[pt], w=[ge])
        P.tt(d1.t[:], mid.t[:], lo.t[:], ALU.subtract, r=[mid, lo], w=[d1])
        P.tt(d1.t[:], d1.t[:], ge.t[:], ALU.mult, r=[d1, ge], w=[d1])
        P.tt(lo.t[:], lo.t[:], d1.t[:], ALU.add, r=[lo, d1], w=[lo])
        P.tt(d1.t[:], hi.t[:], mid.t[:], ALU.subtract, r=[hi, mid], w=[d1])
        P.tt(d1.t[:], d1.t[:], ge.t[:], ALU.mult, r=[d1, ge], w=[d1])
        P.tt(hi.t[:], mid.t[:], d1.t[:], ALU.add, r=[mid, d1], w=[hi])
    P.thr = lo
    mask = cmp
    P.tt(mask.t[:], A.t[:], lo.t[:].unsqueeze(2).to_broadcast([128, 16, 64]), ALU.is_ge, r=[A, lo], w=[mask])
    maskb = k.sb("maskb", [128, 1024], BF16)
    P.cp(maskb.t[:], mask.t[:].rearrange("p e f -> p (e f)"), r=[mask], w=[maskb])
    pre = k.sb("pre5", [128, 16, 64], F32)
    s0 = k.sb("s0", [128, 16, 64], F32); s1 = k.sb("s1", [128, 16, 64], F32)
    for hh in range(2):
        pp, pc = P.nps(), P.nps()
        P.mm(pp.t[:], Ub.t[:], maskb.t[:, hh * 512:(hh + 1) * 512], True, True, r=[Ub, maskb], w=[pp])
        P.mm(pc.t[:], P.ones_b.t[:], maskb.t[:, hh * 512:(hh + 1) * 512], True, True, r=[P.ones_b, maskb], w=[pc])
        P.cp(pre.t[:, hh * 8:(hh + 1) * 8, :].rearrange("p e f -> p (e f)"), pp.t[:], r=[pp], w=[pre])
        P.cp(s0.t[:, hh * 8:(hh + 1) * 8, :].rearrange("p e f -> p (e f)"), pc.t[:], r=[pc], w=[s0], eng="scalar")
    P.tt(pre.t[:], pre.t[:], s0.t[:], ALU.subtract, r=[pre, s0], w=[pre])
    cur, nxt = s0, s1
    for sh in [1, 2, 4, 8, 16, 32]:
        P.cp(nxt.t[:, :, 0:sh], cur.t[:, :, 0:sh], r=[cur], w=[nxt], eng="gpsimd")
        P.tt(nxt.t[:, :, sh:64], cur.t[:, :, sh:64], cur.t[:, :, 0:64 - sh], ALU.add, r=[cur], w=[nxt])
        cur, nxt = nxt, cur
    P.tt(pre.t[:], pre.t[:], cur.t[:], ALU.add, r=[pre, cur], w=[pre])
    P.ts(pre.t[:], pre.t[:], -float(CAP), None, ALU.add, None, r=[pre], w=[pre])
    P.tt(pre.t[:], pre.t[:], mask.t[:], ALU.mult, r=[pre, mask], w=[pre])
    P.ts(pre.t[:], pre.t[:], float(CAP), float(CAP), ALU.add, ALU.min, r=[pre], w=[pre])
    posI = k.sb("posI", [128, 64, 16], I32)
    P.cp(posI.t[:].rearrange("p f e -> p e f"), pre.t[:], r=[pre], w=[posI])
    k.dma("sync", P.pos_scr.rearrange("(f p) e -> p f e", p=128), posI.t[:], r=[posI], w=[P.pos_l])
    myp = k.sb("myp", [128, 64, 4], I32)
    P.dyn_dma("scalar", myp.t[:], lambda base: P.pos_scr[:, bass.ds(base, 4)].rearrange("(f p) e -> p f e", p=128), r=[P.pos_l], w=[myp], mult=4)
    u2t = [k.sb(f"u2g{i}", [128, D], BF16) for i in range(3)]
    for f in range(64):
        ut = u2t[f % 3]
        r_, q_, tt2 = f // 16, (f % 16) // 4, f % 4
        k.dma("sync", ut.t[:], P.u2_all[q_][r_ * 512 + tt2 * 128:r_ * 512 + (tt2 + 1) * 128, :], r=[P.u2a_l[q_]], w=[ut])
        for i in range(4):
            k.custom_dma("gpsimd", lambda e, ut=ut, f=f, i=i: e.indirect_dma_start(
                out=xe_dram[i][:, :], out_offset=bass.IndirectOffsetOnAxis(ap=myp.t[:, f, i:i + 1], axis=0),
                in_=ut.t[:, :], in_offset=None), r=[ut, myp], w=[xe_l[i]])
    k.release(mark)
    if P.dbg and P.stage == 5:
        P.dump("thr", lo, lo.t[0:1, :], [1, 16])
    mark = k.mark()
    xe = k.sb("xe", [128, 8, D], BF16)
    xeT = k.sb("xeT", [128, 8, CAP], BF16)
    hT = k.sb("hT", [128, 8, CAP], BF16)
    sil = [k.sb(f"sil{i}", [128, 512], F32) for i in range(2)]
    yeb = [k.sb(f"yeb{i}", [128, D], BF16) for i in range(2)]
    WB = [[k.sb(f"w{n}b{q}", [128, 8, D], BF16) for n in "gud"] for q in range(2)]
    wst = [k.sb(f"wst{i}", [128, D], F32) for i in range(3)]
    wi = 0

    def load_w_steps(i):
        nonlocal wi
        for (wsrc, wdst) in zip((wg, wu, wd), WB[i % 2]):
            wv_ = wsrc[i].rearrange("(k p) n -> p k n", p=128)
            for kc in range(8):
                st = wst[wi % 3]
                k.dma("sync", st.t[:], wv_[:, kc, :], w=[st])
                P.cp(wdst.t[:, kc, :], st.t[:], r=[st], w=[wdst], eng=["vector", "gpsimd", "scalar"][wi % 3])
                wi += 1
                yield

    for _ in load_w_steps(0):
        pass
    pref = iter(())
    for i in range(4):
        wgb, wub, wdb = WB[i % 2]
        k.dma("sync", xe.t[:], xe_dram[i][0:CAP, :].rearrange("(s p) d -> p s d", p=128), r=[xe_l[i]], w=[xe])
        pref = load_w_steps(i + 1) if i + 1 < 4 else iter(())
        for kc in range(8):
            ps = P.nps()
            psb = ps.t[:].bitcast(BF16)
            for st_ in range(8):
                P.tr(psb[:, st_ * 128:(st_ + 1) * 128], xe.t[:, st_, kc * 128:(kc + 1) * 128], P.ident_b.t[:], r=[xe, P.ident_b], w=[ps])
            P.cp(xeT.t[:, kc, :], psb[:, :], r=[ps], w=[xeT], eng=("scalar" if kc % 2 else "vector"))
        for fc in range(8):
            for hf in range(2):
                hs = slice(hf * 512, (hf + 1) * 512)
                pg, pu = P.nps(), P.nps()
                for kc in range(8):
                    P.mm(pg.t[:], wgb.t[:, kc, fc * 128:(fc + 1) * 128], xeT.t[:, kc, hs], kc == 0, kc == 7, r=[wgb, xeT], w=[pg])
                for kc in range(8):
                    P.mm(pu.t[:], wub.t[:, kc, fc * 128:(fc + 1) * 128], xeT.t[:, kc, hs], kc == 0, kc == 7, r=[wub, xeT], w=[pu])
                sl_ = sil[(fc * 2 + hf) % 2]
                P.act(sl_.t[:], pg.t[:], AF.Silu, r=[pg], w=[sl_])
                P.tt(hT.t[:, fc, hs], sl_.t[:], pu.t[:], ALU.mult, r=[sl_, pu], w=[hT])
                next(pref, None)
        for st_ in range(8):
            yb = yeb[st_ % 2]
            for hh in range(2):
                py = P.nps()
                for fc in range(8):
                    P.mm(py.t[:], hT.t[:, fc, st_ * 128:(st_ + 1) * 128], wdb.t[:, fc, hh * 512:(hh + 1) * 512], fc == 0, fc == 7, r=[hT, wdb], w=[py])
                P.cp(yb.t[:, hh * 512:(hh + 1) * 512], py.t[:], r=[py], w=[yb], eng=("scalar" if hh else "vector"))
                next(pref, None)
            sh = st_ // 4
            k.dma("sync", ye_bounce[i][sh][(st_ % 4) * 128:(st_ % 4 + 1) * 128, :], yb.t[:], r=[yb], w=[yeb_l[i][sh]])
        for _ in pref:
            pass
        for sh in range(2):
            k.collective("AllGather", ALU.bypass, GR, ins=[ye_bounce[i][sh]], outs=[ye_allc[i][sh]], r=[yeb_l[i][sh]], w=[yec_l[i][sh]])
    for i in range(4):
        for sh in range(2):
            k.dma("sync", P.ye_full[i].rearrange("(r s q) d -> r s q d", r=4, s=2)[:, sh],
                  ye_allc[i][sh].rearrange("(r q) d -> r q d", r=4), r=[yec_l[i][sh]], w=[P.yea_l[i]])
    k.release(mark)


def phase6(P):
    k = P.k
    ln2g = P.din("ln2g", [1, D]); ln2b = P.din("ln2b", [1, D])
    y = P.dout("y", [SO, D])
    mark = k.mark()
    ln2g_bc = P.load("ln2g_bc", [128, D], ln2g.to_broadcast([128, D]))
    ln2b_bc = P.load("ln2b_bc", [128, D], ln2b.to_broadcast([128, D]))
    posO = k.sb("posO", [128, 16, 16], I32)
    P.dyn_dma("sync", posO.t[:], lambda base: P.pos_scr[bass.ds(base, SO), :].rearrange("(t p) e -> p t e", p=128), r=[P.pos_l], w=[posO])
    posF = k.sb("posF", [128, 16, 16], F32)
    P.cp(posF.t[:], posO.t[:], r=[posO], w=[posF])
    gw = k.sb("gw", [128, 16, 16], F32)
    P.ts(gw.t[:], posF.t[:], CAP - 0.5, None, ALU.is_lt, None, r=[posF], w=[gw])
    P.tt(posF.t[:], posF.t[:], gw.t[:], ALU.mult, r=[posF, gw], w=[posF])
    P.tt(gw.t[:], gw.t[:], P.aff_own.t[:], ALU.mult, r=[gw, P.affo_l], w=[gw])
    for r_ in range(1, 4):
        P.ts(posF.t[:, :, 4 * r_:4 * r_ + 4], posF.t[:, :, 4 * r_:4 * r_ + 4], float(r_ * CAP), None, ALU.add, None, r=[posF], w=[posF])
    rowI = k.sb("rowI", [128, 16, 16], I32)
    P.cp(rowI.t[:], posF.t[:], r=[posF], w=[rowI])
    NB = 8
    G = [k.sb(f"G{i}", [128, D], BF16) for i in range(NB)]
    for g_ in G:
        P.memset(g_.t[:], 0.0, w=[g_])
    acc = k.sb("acc6", [128, D], F32)
    x1t = [k.sb(f"x1r{i}", [128, D], F32) for i in range(2)]
    v = k.sb("v6", [128, D], F32)
    junk = k.sb("junk6", [128, D], F32)
    st = k.sb("st6", [128, 8], F32)
    outt = [k.sb(f"out{i}", [128, D], F32) for i in range(2)]
    gi = 0
    for t in range(16):
        xt_ = x1t[t % 2]
        k.dma("sync", xt_.t[:], P.x1_scr[t * 128:(t + 1) * 128, :], r=[P.x1_l[t]], w=[xt_])
        P.memset(acc.t[:], 0.0, w=[acc], eng="gpsimd")
        for e_ in range(16):
            g_ = G[gi % NB]
            gi += 1
            i_loc = e_ % 4
            k.custom_dma("gpsimd", lambda e, g_=g_, t=t, e_=e_, i_loc=i_loc: e.indirect_dma_start(
                out=g_.t[:, :], out_offset=None,
                in_=P.ye_full[i_loc][:, :], in_offset=bass.IndirectOffsetOnAxis(ap=rowI.t[:, t, e_:e_ + 1], axis=0),
                ), r=[P.yea_l[i_loc], rowI, g_], w=[g_])
            P.stt(acc.t[:], g_.t[:], gw.t[:, t, e_:e_ + 1], acc.t[:], ALU.mult, ALU.add, r=[g_, gw, acc], w=[acc])
        P.tt(junk.t[:], acc.t[:], P.modR.t[:, 3, :], ALU.mult, r=[acc, P.modR], w=[junk], eng="gpsimd")
        P.stt(v.t[:], xt_.t[:], ALPHA, junk.t[:], ALU.mult, ALU.add, r=[xt_, junk], w=[v])
        ot = outt[t % 2]
        P.layer_norm(ot, v, ln2g_bc, ln2b_bc, st, junk)
        k.dma("sync", y[t * 128:(t + 1) * 128, :], ot.t[:], r=[ot], final=True)
    k.release(mark)


_CACHE = {}


def kernel(**inputs):
    inp = {k_: np.asarray(v) for k_, v in inputs.items()}
    if "prog" not in _CACHE:
        P = Prog(stage=9, dbg=False)
        P.hc = host_consts()
        build_all(P)
        P.k.emit()
        _CACHE["prog"] = P
    P = _CACHE["prog"]
    in_maps = []
    for c in range(8):
        hp = host_prep(inp, c)
        hp.update(P.hc)
        in_maps.append({n: np.ascontiguousarray(hp[n]) for n in P.inp})
    res = run_bass_kernel_spmd(P.nc, in_maps, core_ids=list(range(8)))
    out = np.zeros((2, S, D), np.float32)
    for c in range(8):
        b, j = c // 4, c % 4
        out[b, j * SO:(j + 1) * SO, :] = np.asarray(res.results[c]["y"], dtype=np.float32)
    return out
```
